# Optimizing a Trainium2 kernel written in Bass

```python
import jax, jax.numpy as jnp
from jax import lax
import numpy as np

D_MODEL = 1024
BATCH = 8
SEQ = 4096
DEPTH = 2

CHUNK = 64
N_MIXERS = 2
N_GLA = (DEPTH + N_MIXERS - 1) // N_MIXERS
N_SSD = DEPTH // N_MIXERS
EPS = 1e-6

GLA_HEADS = 4
GLA_DK = D_MODEL // 2
GLA_DV = D_MODEL
GLA_HK = GLA_DK // GLA_HEADS
GLA_HV = GLA_DV // GLA_HEADS
GLA_GATE_RANK = 16
GLA_GATE_TAU = 16.0
GLA_IN = 2 * GLA_DK + 2 * GLA_DV + GLA_GATE_RANK

SSD_INNER = 2 * D_MODEL
SSD_HEADDIM = 64
SSD_HEADS = SSD_INNER // SSD_HEADDIM
SSD_GROUPS = 4
SSD_HPG = SSD_HEADS // SSD_GROUPS
SSD_STATE = 128
SSD_CONV = 4
SSD_CONV_CH = SSD_INNER + 2 * SSD_GROUPS * SSD_STATE
SSD_IN = SSD_INNER + SSD_CONV_CH + SSD_HEADS

N_EXPERTS = 16
N_GROUPS = 4
EXPERTS_PER_GROUP = N_EXPERTS // N_GROUPS
TOPK_GROUP = 1
TOP_K = 2
D_EXPERT = 512

kernel_name = 'hybrid_gla_ssd_moe_adaln'


def rms_normalize(x):
    xf = x.astype(jnp.float32)
    return xf * lax.rsqrt(jnp.mean(xf * xf, axis=-1, keepdims=True) + EPS)


def ada_norm(x, g, shift, scale):
    y = rms_normalize(x) * g.astype(jnp.float32)
    y = y * (1.0 + scale[:, None, :].astype(jnp.float32)) + shift[:, None, :].astype(jnp.float32)
    return y.astype(x.dtype)


def gla_mixer(h, w_in, w_gate2, b_gate2, norm_g, w_out):
    bsz, s, _ = h.shape
    n = s // CHUNK
    proj = h @ w_in
    q, k, v, r, g_lr = jnp.split(proj, [GLA_DK, 2 * GLA_DK, 2 * GLA_DK + GLA_DV, 2 * GLA_DK + 2 * GLA_DV], axis=-1)
    log_a = jax.nn.log_sigmoid((g_lr @ w_gate2 + b_gate2).astype(jnp.float32)) / GLA_GATE_TAU

    def to_chunks(t, d):
        return t.reshape(bsz, n, CHUNK, GLA_HEADS, d).transpose(1, 0, 3, 2, 4).astype(jnp.float32)

    qc = to_chunks(q, GLA_HK) * (GLA_HK ** -0.5)
    kc = to_chunks(k, GLA_HK)
    vc = to_chunks(v, GLA_HV)
    bc = jnp.cumsum(to_chunks(log_a, GLA_HK), axis=3)
    causal = jnp.tril(jnp.ones((CHUNK, CHUNK), dtype=bool))

    def step(state, xs):
        q_c, k_c, v_c, b_c = xs
        diff = b_c[:, :, :, None, :] - b_c[:, :, None, :, :]
        decay = jnp.exp(jnp.where(causal[None, None, :, :, None], diff, -jnp.inf))
        scores = jnp.sum(q_c[:, :, :, None, :] * k_c[:, :, None, :, :] * decay, axis=-1)
        b_last = b_c[:, :, -1, :]
        o = jnp.einsum('bhts,bhsv->bhtv', scores, v_c) + jnp.einsum('bhtd,bhdv->bhtv', q_c * jnp.exp(b_c), state)
        k_end = k_c * jnp.exp(b_last[:, :, None, :] - b_c)
        state = jnp.exp(b_last)[..., None] * state + jnp.einsum('bhsd,bhsv->bhdv', k_end, v_c)
        return state, o

    state0 = jnp.zeros((bsz, GLA_HEADS, GLA_HK, GLA_HV), jnp.float32)
    _, o = lax.scan(step, state0, (qc, kc, vc, bc))
    o = o.transpose(1, 0, 3, 2, 4).reshape(bsz, s, GLA_HEADS, GLA_HV)
    o = rms_normalize(o) * norm_g.astype(jnp.float32)
    o = o.reshape(bsz, s, GLA_DV) * jax.nn.silu(r.astype(jnp.float32))
    return o.astype(h.dtype) @ w_out


def causal_depthwise_conv(u, w):
    return lax.conv_general_dilated(
        u, w[:, None, :].astype(u.dtype), window_strides=(1,), padding=[(w.shape[0] - 1, 0)],
        dimension_numbers=('NWC', 'WIO', 'NWC'), feature_group_count=u.shape[-1])


def ssd_mixer(h, w_in, conv_w, conv_b, dt_bias, a_log, d_skip, norm_g, w_out):
    bsz, s, _ = h.shape
    n = s // CHUNK
    proj = h @ w_in
    z, xbc, dt = jnp.split(proj, [SSD_INNER, SSD_INNER + SSD_CONV_CH], axis=-1)
    xbc = jax.nn.silu(causal_depthwise_conv(xbc, conv_w) + conv_b)
    xs_, b_, c_ = jnp.split(xbc, [SSD_INNER, SSD_INNER + SSD_GROUPS * SSD_STATE], axis=-1)
    dt = jax.nn.softplus(dt.astype(jnp.float32) + dt_bias.astype(jnp.float32))
    a = -jnp.exp(a_log.astype(jnp.float32))
    xh = xs_.reshape(bsz, s, SSD_HEADS, SSD_HEADDIM).astype(jnp.float32)

    def to_chunks(t):
        return jnp.moveaxis(t.reshape(bsz, n, CHUNK, *t.shape[2:]), 1, 0)

    bg = b_.reshape(bsz, s, SSD_GROUPS, SSD_STATE).astype(jnp.float32)
    cg = c_.reshape(bsz, s, SSD_GROUPS, SSD_STATE).astype(jnp.float32)
    causal = jnp.tril(jnp.ones((CHUNK, CHUNK), dtype=bool))

    def step(state, xs):
        x_c, dt_c, b_c, c_c = xs
        acum = jnp.cumsum(dt_c * a, axis=1)
        seg = acum[:, :, None, :] - acum[:, None, :, :]
        lmat = jnp.exp(jnp.where(causal[None, :, :, None], seg, -jnp.inf))
        cb = jnp.repeat(jnp.einsum('btgn,bsgn->btsg', c_c, b_c), SSD_HPG, axis=-1)
        xdt = x_c * dt_c[..., None]
        y_diag = jnp.einsum('btsh,bshp->bthp', cb * lmat, xdt)
        c_h = jnp.repeat(c_c, SSD_HPG, axis=2)
        b_h = jnp.repeat(b_c, SSD_HPG, axis=2)
        y_off = jnp.einsum('bthn,bhpn->bthp', c_h, state) * jnp.exp(acum)[..., None]
        decay_end = jnp.exp(acum[:, -1:, :] - acum)
        state = jnp.exp(acum[:, -1, :])[:, :, None, None] * state + jnp.einsum('bshn,bshp->bhpn', b_h * decay_end[..., None], xdt)
        return state, y_diag + y_off

    state0 = jnp.zeros((bsz, SSD_HEADS, SSD_HEADDIM, SSD_STATE), jnp.float32)
    _, y = lax.scan(step, state0, (to_chunks(xh), to_chunks(dt), to_chunks(bg), to_chunks(cg)))
    y = jnp.moveaxis(y, 0, 1).reshape(bsz, s, SSD_HEADS, SSD_HEADDIM)
    y = y + d_skip.astype(jnp.float32)[:, None] * xh
    y = y.reshape(bsz, s, SSD_INNER) * jax.nn.silu(z.astype(jnp.float32))
    y = rms_normalize(y.reshape(bsz, s, SSD_GROUPS, SSD_INNER // SSD_GROUPS)).reshape(bsz, s, SSD_INNER)
    y = y * norm_g.astype(jnp.float32)
    return y.astype(h.dtype) @ w_out


def moe_ffn(h, router_w, router_b, w_gate, w_up, w_down):
    bsz, s, d = h.shape
    t = h.reshape(-1, d)
    ntok = t.shape[0]
    scores = jax.nn.sigmoid((t @ router_w).astype(jnp.float32))
    biased = scores + router_b.astype(jnp.float32)
    group_score = lax.top_k(biased.reshape(ntok, N_GROUPS, EXPERTS_PER_GROUP), 2)[0].sum(-1)
    _, gidx = lax.top_k(group_score, TOPK_GROUP)
    gmask = jax.nn.one_hot(gidx, N_GROUPS, dtype=jnp.float32).sum(1) > 0
    emask = jnp.repeat(gmask, EXPERTS_PER_GROUP, axis=1)
    _, eidx = lax.top_k(jnp.where(emask, biased, -jnp.inf), TOP_K)
    w_sel = jnp.take_along_axis(scores, eidx, axis=1)
    w_sel = w_sel / jnp.sum(w_sel, axis=-1, keepdims=True)
    gates = jnp.sum(jax.nn.one_hot(eidx, N_EXPERTS, dtype=jnp.float32) * w_sel[..., None], axis=1)
    y = jnp.zeros((ntok, d), jnp.float32)
    for e in range(N_EXPERTS):
        he = jax.nn.silu(t @ w_gate[e]) * (t @ w_up[e])
        y = y + gates[:, e:e + 1] * (he @ w_down[e]).astype(jnp.float32)
    return y.astype(h.dtype).reshape(bsz, s, d)


def setup_inputs(seed: int = 0) -> dict:
    key = jax.random.key(seed)
    ks = jax.random.split(key, 32)
    f32 = jnp.float32

    def nrm(k, shape, scale):
        return jax.random.normal(k, shape, f32) * scale

    dt0 = jnp.exp(jax.random.uniform(ks[14], (N_SSD, SSD_HEADS), f32, np.log(1e-3), np.log(1e-1)))
    return {
        'x': nrm(ks[0], (BATCH, SEQ, D_MODEL), 1.0),
        'c': nrm(ks[1], (BATCH, D_MODEL), 1.0),
        'ada_w': nrm(ks[2], (DEPTH, D_MODEL, 6 * D_MODEL), 0.5 * D_MODEL ** -0.5),
        'ada_b': nrm(ks[3], (DEPTH, 6 * D_MODEL), 0.02),
        'norm_mix': 1.0 + nrm(ks[4], (DEPTH, D_MODEL), 0.02),
        'norm_ffn': 1.0 + nrm(ks[5], (DEPTH, D_MODEL), 0.02),
        'norm_final': 1.0 + nrm(ks[6], (D_MODEL,), 0.02),
        'gla_w_in': nrm(ks[7], (N_GLA, D_MODEL, GLA_IN), D_MODEL ** -0.5),
        'gla_w_gate2': nrm(ks[8], (N_GLA, GLA_GATE_RANK, GLA_DK), GLA_GATE_RANK ** -0.5),
        'gla_b_gate2': nrm(ks[9], (N_GLA, GLA_DK), 0.1),
        'gla_norm': 1.0 + nrm(ks[10], (N_GLA, GLA_HV), 0.02),
        'gla_w_out': nrm(ks[11], (N_GLA, GLA_DV, D_MODEL), GLA_DV ** -0.5),
        'ssd_w_in': nrm(ks[12], (N_SSD, D_MODEL, SSD_IN), D_MODEL ** -0.5),
        'ssd_conv_w': nrm(ks[13], (N_SSD, SSD_CONV, SSD_CONV_CH), SSD_CONV ** -0.5),
        'ssd_conv_b': nrm(ks[15], (N_SSD, SSD_CONV_CH), 0.02),
        'ssd_dt_bias': dt0 + jnp.log(-jnp.expm1(-dt0)),
        'ssd_a_log': jnp.log(jax.random.uniform(ks[16], (N_SSD, SSD_HEADS), f32, 1.0, 16.0)),
        'ssd_d': 1.0 + nrm(ks[17], (N_SSD, SSD_HEADS), 0.1),
        'ssd_norm': 1.0 + nrm(ks[18], (N_SSD, SSD_INNER), 0.02),
        'ssd_w_out': nrm(ks[19], (N_SSD, SSD_INNER, D_MODEL), SSD_INNER ** -0.5),
        'router_w': nrm(ks[20], (D_MODEL, N_EXPERTS), D_MODEL ** -0.5),
        'router_b': nrm(ks[21], (N_EXPERTS,), 0.01),
        'moe_w_gate': nrm(ks[22], (DEPTH, N_EXPERTS, D_MODEL, D_EXPERT), D_MODEL ** -0.5),
        'moe_w_up': nrm(ks[23], (DEPTH, N_EXPERTS, D_MODEL, D_EXPERT), D_MODEL ** -0.5),
        'moe_w_down': nrm(ks[24], (DEPTH, N_EXPERTS, D_EXPERT, D_MODEL), D_EXPERT ** -0.5),
    }


def reference(x, c, ada_w, ada_b, norm_mix, norm_ffn, norm_final,
              gla_w_in, gla_w_gate2, gla_b_gate2, gla_norm, gla_w_out,
              ssd_w_in, ssd_conv_w, ssd_conv_b, ssd_dt_bias, ssd_a_log, ssd_d, ssd_norm, ssd_w_out,
              router_w, router_b, moe_w_gate, moe_w_up, moe_w_down):
    cond = jax.nn.silu(c)
    for i in range(DEPTH):
        mod = cond @ ada_w[i] + ada_b[i]
        sh1, sc1, g1, sh2, sc2, g2 = jnp.split(mod, 6, axis=-1)
        h = ada_norm(x, norm_mix[i], sh1, sc1)
        j = i // N_MIXERS
        if i % N_MIXERS == 0:
            mix = gla_mixer(h, gla_w_in[j], gla_w_gate2[j], gla_b_gate2[j], gla_norm[j], gla_w_out[j])
        else:
            mix = ssd_mixer(h, ssd_w_in[j], ssd_conv_w[j], ssd_conv_b[j], ssd_dt_bias[j], ssd_a_log[j],
                            ssd_d[j], ssd_norm[j], ssd_w_out[j])
        x = x + g1[:, None, :] * mix
        h = ada_norm(x, norm_ffn[i], sh2, sc2)
        x = x + g2[:, None, :] * moe_ffn(h, router_w, router_b, moe_w_gate[i], moe_w_up[i], moe_w_down[i])
    return (rms_normalize(x) * norm_final.astype(jnp.float32)).astype(x.dtype)
```

```python
import contextlib
import numpy as np
import concourse.bass as bass
import concourse.mybir as mybir
from concourse.bass_utils import run_bass_kernel_spmd

F32 = mybir.dt.float32
BF16 = mybir.dt.bfloat16
AF = mybir.ActivationFunctionType
ALU = mybir.AluOpType
AX = mybir.AxisListType

D = 1024
NCH = 8
TB = 512
EPS = 1e-6
NE = 16
DE = 512
SAME_ENGINE_SYNC = True


class Sem:
    def __init__(self, h, owner=None):
        self.h = h
        self.owner = owner
        self.val = 0


class Buf:
    __slots__ = ("name", "w", "r", "dsem")

    def __init__(self, name):
        self.name = name
        self.w = None
        self.r = {}
        self.dsem = None


class Ctx:
    def __init__(self, nc, es):
        self.nc = nc
        self.es = es
        self.eng = {"pe": nc.tensor, "act": nc.scalar, "dve": nc.vector, "pool": nc.gpsimd, "sp": nc.sync}
        self.sem = {k: Sem(es.enter_context(nc.semaphore("s_" + k)), owner=k) for k in self.eng}
        self.seen = {k: {} for k in self.eng}
        self.nsem = 0

    def _wait(self, e, reads, writes):
        need = {}
        for b in list(reads) + list(writes):
            if b.w is not None:
                s, v, big = b.w
                if s.owner == e and (e == "pe" or big or not SAME_ENGINE_SYNC):
                    continue
                need[s] = max(need.get(s, 0), v)
        for b in writes:
            for s, v in b.r.items():
                if s.owner == e:
                    continue
                need[s] = max(need.get(s, 0), v)
        for s, v in need.items():
            if self.seen[e].get(s, 0) >= v:
                continue
            self.eng[e].wait_ge(s.h, v)
            self.seen[e][s] = v

    def _rec(self, s, v, reads, writes, big=False):
        for b in reads:
            if b.r.get(s, 0) < v:
                b.r[s] = v
        for b in writes:
            b.w = (s, v, big)
            b.r = {}

    def op(self, e, reads, writes, fn, big=False):
        self._wait(e, reads, writes)
        ins = fn(self.eng[e])
        s = self.sem[e]
        s.val += 1
        ins.then_inc(s.h, 1)
        self._rec(s, s.val, reads, writes, big)
        return ins

    @contextlib.contextmanager
    def group(self, e, reads, writes):
        self._wait(e, reads, writes)
        box = []
        yield box
        s = self.sem[e]
        s.val += 1
        box[-1].then_inc(s.h, 1)
        self._rec(s, s.val, reads, writes)

    def dma(self, q, out, in_, reads, writes, on):
        self._wait(q, reads, writes)
        if on.dsem is None:
            self.nsem += 1
            on.dsem = Sem(self.es.enter_context(self.nc.semaphore("d%d" % self.nsem)), owner=None)
        ins = self.eng[q].dma_start(out=out, in_=in_)
        s = on.dsem
        s.val += 16
        ins.then_inc(s.h, 16)
        self._rec(s, s.val, reads, writes)
        return ins

    def barrier(self, bufs=()):
        for e in self.eng:
            for k, s in self.sem.items():
                if k == e or s.val == 0:
                    continue
                if self.seen[e].get(s, 0) >= s.val:
                    continue
                self.eng[e].wait_ge(s.h, s.val)
                self.seen[e][s] = s.val
        for b in bufs:
            need = {}
            if b.w is not None and b.w[0].owner is None:
                need[b.w[0]] = b.w[1]
            for s, v in b.r.items():
                if s.owner is None:
                    need[s] = max(need.get(s, 0), v)
            for e in self.eng:
                for s, v in need.items():
                    if self.seen[e].get(s, 0) < v:
                        self.eng[e].wait_ge(s.h, v)
                        self.seen[e][s] = v


def run_threads(gens, weights=None):
    active = [(g, (weights[i] if weights else 1)) for i, g in enumerate(gens)]
    while active:
        for item in list(active):
            g, w = item
            for _ in range(w):
                try:
                    next(g)
                except StopIteration:
                    active.remove(item)
                    break


class K:
    def __init__(self, ntok, debug=False):
        self.ntok = ntok
        self.nblk = ntok // TB
        self.debug = debug
        self.mask_base = 0
        self.nc = bass.Bass("TRN2", target_bir_lowering=False)
        self.es = contextlib.ExitStack()

    def sb(self, es, name, shape, dt=F32):
        self._uid = getattr(self, "_uid", 0) + 1
        return es.enter_context(self.nc.sbuf_tensor("%s_%d" % (name, self._uid), list(shape), dt))

    def tap(self, name, ap, bufs, dt=F32):
        if not self.debug:
            return
        d = self.nc.dram_tensor("dbg_" + name, list(ap.shape), dt, kind="ExternalOutput").ap()
        b = Buf("dbg_" + name)
        self.cx.dma("sp", d, ap, list(bufs), [b], b)
        self.nc.sync.wait_ge(b.dsem.h, b.dsem.val)

    def din(self, name, shape, dt=F32):
        return self.nc.dram_tensor(name, list(shape), dt, kind="ExternalInput").ap()

    def build(self):
        nc = self.nc
        ntok = self.ntok
        with self.es as es:
            self.cx = cx = Ctx(nc, es)
            I = self.I = {}
            I["x"] = self.din("x", [ntok, D])
            I["c_col"] = self.din("c_col", [128, 8])
            I["ada_w"] = self.din("ada_w", [2, D, 6 * D])
            I["ada_b"] = self.din("ada_b", [2, 1, 6 * D])
            I["norm_mix"] = self.din("norm_mix", [2, 128, 8])
            I["norm_ffn"] = self.din("norm_ffn", [2, 128, 8])
            I["norm_final"] = self.din("norm_final", [128, 8])
            I["gla_w_in"] = self.din("gla_w_in", [D, 3088])
            I["gla_w_gate2"] = self.din("gla_w_gate2", [16, 512])
            I["gla_b_gate2"] = self.din("gla_b_gate2", [128, 4])
            I["gla_norm"] = self.din("gla_norm", [128, 2])
            I["gla_w_out"] = self.din("gla_w_out", [D, D])
            I["ssd_w_in"] = self.din("ssd_w_in", [D, 5152])
            I["ssd_conv_w"] = self.din("ssd_conv_w", [128, 24, 4])
            I["ssd_conv_b"] = self.din("ssd_conv_b", [128, 24])
            I["ssd_dt_bias"] = self.din("ssd_dt_bias", [32, 1])
            I["ssd_a_log"] = self.din("ssd_a_log", [32, 1])
            I["ssd_d_bc"] = self.din("ssd_d_bc", [128, 32])
            I["ssd_norm_col"] = self.din("ssd_norm_col", [128, 16])
            I["ssd_w_out"] = self.din("ssd_w_out", [2048, D])
            I["router_w"] = self.din("router_w", [D, 16])
            I["router_b_bc"] = self.din("router_b_bc", [128, 16])
            I["moe_w_gate"] = self.din("moe_w_gate", [2, NE, D, DE])
            I["moe_w_up"] = self.din("moe_w_up", [2, NE, D, DE])
            I["moe_w_down"] = self.din("moe_w_down", [2, NE, DE, D])
            self.out = nc.dram_tensor("out", [ntok, D], F32, kind="ExternalOutput").ap()
            kind = "ExternalOutput" if self.debug else "Internal"
            self.xs = nc.dram_tensor("xs", [NCH, 128, ntok], F32, kind=kind).ap()
            self.modrow = nc.dram_tensor("modrow", [2, 6 * D], F32, kind=kind).ap()
            self.xs_buf = [Buf("xs%d" % b) for b in range(self.nblk)]
            self.out_buf = Buf("out")

            self.ps = [es.enter_context(nc.psum_tensor("ps%d" % i, [128, 512], F32)) for i in range(8)]
            self.psb = [Buf("psb%d" % i) for i in range(8)]

            self.consts(es)
            self.phase_mod(0)
            self.phase_mod(1)
            self.phase_xin()
            cx.barrier(self.xs_buf)
            self.phases()
            s = self.out_buf.dsem
            if s is not None:
                nc.sync.wait_ge(s.h, s.val)
        return nc

    def consts(self, es):
        nc, cx = self.nc, self.cx
        self.ident_f = self.sb(es, "ident_f", [128, 128], F32)
        self.ident_b = self.sb(es, "ident_b", [128, 128], BF16)
        self.ones_b = self.sb(es, "ones_b", [128, 128], BF16)
        self.cbuf = Buf("consts")
        g = nc.gpsimd
        g.memset(self.ident_f[:], 1.0)
        g.affine_select(out=self.ident_f[:], in_=self.ident_f[:], pattern=[[-1, 128]],
                        compare_op=ALU.is_equal, fill=0.0, base=0, channel_multiplier=1)
        g.tensor_copy(out=self.ident_b[:], in_=self.ident_f[:])
        self.eps_t = self.sb(es, "eps_t", [128, 1], F32)
        g.memset(self.eps_t[:], EPS)
        cx.op("pool", [], [self.cbuf], lambda e: e.memset(self.ones_b[:], 1.0))
        self.mod = self.sb(es, "mod", [128, 2, 48], F32)
        self.mod_buf = [Buf("mod0"), Buf("mod1")]
        self.cond = self.sb(es, "cond", [128, 8], F32)
        self.cond_buf = Buf("cond")
        c_raw = self.sb(es, "c_raw", [128, 8], F32)
        cb = Buf("c_raw")
        cx.dma("sp", c_raw[:], self.I["c_col"], [], [cb], cb)
        cx.op("act", [cb], [self.cond_buf], lambda e: e.activation(out=self.cond[:], in_=c_raw[:], func=AF.Silu))
        self.gains = self.sb(es, "gains", [128, 5, 8], F32)
        self.gains_buf = Buf("gains")
        for i in range(2):
            cx.dma("sp", self.gains[:, i, :], self.I["norm_mix"][i], [], [self.gains_buf], self.gains_buf)
            cx.dma("sp", self.gains[:, 2 + i, :], self.I["norm_ffn"][i], [], [self.gains_buf], self.gains_buf)
        cx.dma("sp", self.gains[:, 4, :], self.I["norm_final"], [], [self.gains_buf], self.gains_buf)
        self.AB = self.sb(es, "AB", [128, 2, 2, 2, 8], F32)
        self.AB_buf = [[Buf("AB%d%d" % (i, j)) for j in range(2)] for i in range(2)]

    def phase_mod(self, layer):
        nc, cx = self.nc, self.cx
        with contextlib.ExitStack() as es:
            wbufs = [self.sb(es, "adaw%d" % i, [128, 8, 512], F32) for i in range(2)]
            wb = [Buf("adaw%d" % i) for i in range(2)]
            brow = self.sb(es, "adab", [1, 6 * D], F32)
            bb = Buf("adab")
            row = self.sb(es, "modrow_sb", [1, 6 * D], F32)
            rb = Buf("modrow_sb")
            cx.dma("sp", brow[:], self.I["ada_b"][layer], [], [bb], bb)
            for j in range(12):
                w = wbufs[j % 2]
                cx.dma("sp", w[:], self.I["ada_w"][layer, :, j * 512:(j + 1) * 512].rearrange("(c p) n -> p c n", p=128),
                       [], [wb[j % 2]], wb[j % 2])
                pb = j % 2
                with cx.group("pe", [wb[j % 2], self.cond_buf], [self.psb[pb]]) as box:
                    for c in range(8):
                        box.append(nc.tensor.matmul(self.ps[pb][0:1, :], lhsT=self.cond[:, c:c + 1], rhs=w[:, c, :],
                                                    start=(c == 0), stop=(c == 7)))
                cx.op("dve", [self.psb[pb], bb], [rb],
                      lambda e: e.tensor_tensor(out=row[0:1, j * 512:(j + 1) * 512], in0=self.ps[pb][0:1, :],
                                                in1=brow[0:1, j * 512:(j + 1) * 512], op=ALU.add))
            mrb = Buf("modrow_dram")
            cx.dma("sp", self.modrow[layer:layer + 1, :], row[:], [rb], [mrb], mrb)
            with nc.allow_non_contiguous_dma(reason="tiny mod vector relayout"):
                cx.dma("sp", self.mod[:, layer, :], self.modrow[layer].rearrange("(j p) -> p j", p=128),
                       [mrb], [self.mod_buf[layer]], self.mod_buf[layer])
            for which in range(2):
                gi = (0 if which == 0 else 2) + layer
                sh = self.mod[:, layer, which * 24: which * 24 + 8]
                sc = self.mod[:, layer, which * 24 + 8: which * 24 + 16]
                A = self.AB[:, layer, which, 0, :]
                B = self.AB[:, layer, which, 1, :]
                ab = self.AB_buf[layer][which]
                cx.op("dve", [self.mod_buf[layer], self.gains_buf], [ab],
                      lambda e: e.scalar_tensor_tensor(out=A, in0=sc, scalar=1.0, in1=self.gains[:, gi, :],
                                                       op0=ALU.add, op1=ALU.mult))
                cx.op("dve", [self.mod_buf[layer]], [ab], lambda e: e.tensor_copy(out=B, in_=sh))
            cx.barrier([bb, rb, mrb] + wb)

    def phase_xin(self):
        nc, cx = self.nc, self.cx
        with contextlib.ExitStack() as es:
            xt = [self.sb(es, "xin%d" % i, [128, D], F32) for i in range(2)]
            xtb = [Buf("xin%d" % i) for i in range(2)]
            xT = [self.sb(es, "xT%d" % i, [128, NCH, TB], F32) for i in range(2)]
            xTb = [Buf("xT%d" % i) for i in range(2)]
            for blk in range(self.nblk):
                o = xT[blk % 2]
                ob = xTb[blk % 2]
                for tt in range(4):
                    n = blk * 4 + tt
                    t = xt[n % 2]
                    tb = xtb[n % 2]
                    cx.dma("sp", t[:], self.I["x"][n * 128:(n + 1) * 128, :], [], [tb], tb)
                    for half in range(2):
                        pb = (n * 2 + half) % 4
                        with cx.group("pe", [tb, self.cbuf], [self.psb[pb]]) as box:
                            for q in range(4):
                                c = half * 4 + q
                                box.append(nc.tensor.transpose(out=self.ps[pb][:, q * 128:(q + 1) * 128],
                                                               in_=t[:, c * 128:(c + 1) * 128], identity=self.ident_f[:]))
                        src = self.ps[pb][:].rearrange("p (q t) -> p q t", t=128)
                        dst = o[:, half * 4:(half + 1) * 4, tt * 128:(tt + 1) * 128]
                        if half == 0:
                            cx.op("act", [self.psb[pb]], [ob], lambda e: e.copy(out=dst, in_=src))
                        else:
                            cx.op("dve", [self.psb[pb]], [ob], lambda e: e.tensor_copy(out=dst, in_=src))
                cx.dma("sp", self.xs[:, :, blk * TB:(blk + 1) * TB].rearrange("c p t -> p c t"), o[:],
                       [ob], [self.xs_buf[blk]], self.xs_buf[blk])
            cx.barrier(xtb + xTb)

    def phases(self):
        plan = self.plan if getattr(self, "plan", None) else ["gla", "moe0", "ssd", "moe1", "final"]
        for p in plan:
            if p == "gla":
                self.phase_gla()
            elif p == "ssd":
                self.phase_ssd()
            elif p == "moe0":
                self.phase_moe(0)
            elif p == "moe1":
                self.phase_moe(1)
            elif p == "final":
                self.phase_final()
            self.cx.barrier(self.xs_buf)

    def norm_block(self, *a, **kw):
        for _ in self.norm_block_gen(*a, **kw):
            pass

    def norm_block_gen(self, W, x, xb, A, B, ab, h, hb, sq, sqb, tmp, tmpb, s_t, sb_, pbank, gain_only=False):
        nc, cx = self.nc, self.cx
        cx.op("act", [xb], [sqb], lambda e: e.activation(out=sq, in_=x, func=AF.Square))
        yield
        with cx.group("pe", [sqb, self.cbuf], [self.psb[pbank]]) as box:
            for c in range(8):
                box.append(nc.tensor.matmul(self.ps[pbank][:, 0:W], lhsT=self.ones_b[:], rhs=sq[:, c, :],
                                            start=(c == 0), stop=(c == 7)))
        cx.op("act", [self.psb[pbank]], [sb_],
              lambda e: e.activation(out=s_t[:, 0, 0:W], in_=self.ps[pbank][:, 0:W], func=AF.Sqrt, bias=self.eps_t[:, 0:1], scale=1.0 / D))
        yield
        cx.op("dve", [sb_], [sb_], lambda e: e.reciprocal(out=s_t[:, 1, 0:W], in_=s_t[:, 0, 0:W]), big=True)
        for c in range(8):
            cx.op("dve", [xb, sb_, ab], [tmpb],
                  lambda e: e.scalar_tensor_tensor(out=tmp[:, c, :], in0=x[:, c, :], scalar=A[:, c:c + 1],
                                                   in1=s_t[:, 1, 0:W], op0=ALU.mult, op1=ALU.mult))
        yield
        if gain_only:
            return
        for c in range(8):
            cx.op("act", [tmpb, ab], [hb],
                  lambda e: e.activation(out=h[:, c, :], in_=tmp[:, c, :], func=AF.Identity, bias=B[:, c:c + 1], scale=1.0))

    def phase_moe(self, layer):
        nc, cx = self.nc, self.cx
        HT = min(2048, self.ntok)
        nhalf = self.ntok // HT
        nb = HT // TB
        NT = HT // 128
        W = 256
        A = self.AB[:, layer, 1, 0, :]
        B = self.AB[:, layer, 1, 1, :]
        ab = self.AB_buf[layer][1]
        with contextlib.ExitStack() as es:
            xacc = self.sb(es, "xacc", [128, NCH, HT], F32)
            xab = [Buf("xacc%d" % i) for i in range(nb)]
            h2 = self.sb(es, "h2", [128, NCH, HT], BF16)
            h2b = [Buf("h2_%d" % i) for i in range(nb)]
            wg = [self.sb(es, "wg%d" % i, [128, 8, DE], BF16) for i in range(2)]
            wu = [self.sb(es, "wu%d" % i, [128, 8, DE], BF16) for i in range(2)]
            wd = [self.sb(es, "wd%d" % i, [128, 4, D], BF16) for i in range(2)]
            wgb = [Buf("wg%d" % i) for i in range(2)]
            wub = [Buf("wu%d" % i) for i in range(2)]
            wdb = [Buf("wd%d" % i) for i in range(2)]
            tmp2 = [self.sb(es, "ntmp%d" % i_, [128, NCH, W], F32) for i_ in range(2)]
            tmpb2 = [Buf("ntmp%d" % i_) for i_ in range(2)]
            sq2_ = [self.sb(es, "nsq%d" % i_, [128, NCH, W], BF16) for i_ in range(2)]
            sqb2 = [Buf("nsq%d" % i_) for i_ in range(2)]
            s_t2 = [self.sb(es, "ns%d" % i_, [128, 2, W], F32) for i_ in range(2)]
            sb2_ = [Buf("ns%d" % i_) for i_ in range(2)]
            he = [self.sb(es, "he%d" % i, [128, 4, TB], BF16) for i in range(2)]
            heb = [Buf("he%d" % i) for i in range(2)]
            sg = self.sb(es, "sg", [128, TB], F32)
            sgb = Buf("sg")
            t1 = self.sb(es, "t1", [128, TB], F32)
            t1b = Buf("t1")
            Gs = self.sb(es, "Gs", [128, TB], F32)
            Gsb = Buf("Gs")
            rw = self.sb(es, "rw", [128, NCH, 16], F32)
            rwb = Buf("rw")
            rb = self.sb(es, "rb", [128, 16], F32)
            b16 = self.sb(es, "b16", [16, 1], F32)
            b16b = Buf("b16")
            selt = [self.sb(es, "sel%d" % i_, [16, 128], F32) for i_ in range(2)]
            seltb = [Buf("sel%d" % i_) for i_ in range(2)]
            gT = self.sb(es, "gT", [16, HT], F32)
            gTb = Buf("gT")
            lgT = gT
            lgTb = gTb
            R = [self.sb(es, "rt%d" % i, [128, NT, 16], F32) for i in range(6)]
            Rb = [Buf("rt%d" % i) for i in range(6)]
            Rs = [self.sb(es, "rs%d" % i, [128, NT * 4], F32) for i in range(4)]
            Rsb = [Buf("rs%d" % i) for i in range(4)]

            cx.dma("sp", rw[:], self.I["router_w"].rearrange("(c p) e -> p c e", p=128), [], [rwb], rwb)
            cx.dma("sp", rb[:], self.I["router_b_bc"], [], [rwb], rwb)
            with cx.group("pe", [rwb, ab], [self.psb[6]]) as box:
                for c in range(8):
                    box.append(nc.tensor.matmul(self.ps[6][0:16, 0:1], lhsT=rw[:, c, :], rhs=B[:, c:c + 1], start=(c == 0), stop=(c == 7)))
            cx.op("act", [self.psb[6]], [b16b], lambda e: e.copy(out=b16[:], in_=self.ps[6][0:16, 0:1]))
            ones16 = self.sb(es, "ones16", [16, 128], F32)
            o16b = Buf("ones16")
            cx.op("pool", [], [o16b], lambda e: e.memset(ones16[:], 1.0))

            def load_w(e, half):
                i = (half * NE + e) % 2
                cx.dma("pool", wg[i][:], self.I["moe_w_gate"][layer, e].rearrange("(c p) n -> p c n", p=128), [], [wgb[i]], wgb[i])
                cx.dma("pool", wu[i][:], self.I["moe_w_up"][layer, e].rearrange("(c p) n -> p c n", p=128), [], [wub[i]], wub[i])
                cx.dma("pool", wd[i][:], self.I["moe_w_down"][layer, e].rearrange("(c p) n -> p c n", p=128), [], [wdb[i]], wdb[i])

            for half in range(nhalf):
                t0 = half * HT
                load_w(0, half)
                for blk in range(nb):
                    gb = half * nb + blk
                    cx.dma("sp", xacc[:, :, blk * TB:(blk + 1) * TB],
                           self.xs[:, :, t0 + blk * TB: t0 + (blk + 1) * TB].rearrange("c p t -> p c t"),
                           [self.xs_buf[gb]], [xab[blk]], xab[blk])
                subs = [(blk, sub) for blk in range(nb) for sub in range(TB // W)]
                pend = None
                for n_ in range(len(subs) + 1):
                    cur = None
                    if n_ < len(subs):
                        blk, sub = subs[n_]
                        c0 = blk * TB + sub * W
                        si = n_ % 2
                        tmp, tmpb, sq, sqb, s_t, sb_ = tmp2[si], tmpb2[si], sq2_[si], sqb2[si], s_t2[si], sb2_[si]
                        g_ = self.norm_block_gen(W, xacc[:, :, c0:c0 + W], xab[blk], A, B, ab, h2[:, :, c0:c0 + W], h2b[blk],
                                                 sq[:], sqb, tmp[:], tmpb, s_t, sb_, 4 if si == 0 else 7)
                        for _ in range(3):
                            next(g_)
                        cur = (g_, tmp, tmpb, c0, 6 if si == 0 else 3)
                    if pend is not None:
                        g_, tmp, tmpb, c0, prb = pend
                        for _ in g_:
                            pass
                        with cx.group("pe", [tmpb, rwb], [self.psb[prb]]) as box:
                            for c in range(8):
                                box.append(nc.tensor.matmul(self.ps[prb][0:16, 0:W], lhsT=rw[:, c, :], rhs=tmp[:, c, :], start=(c == 0), stop=(c == 7)))
                        cx.op("act", [self.psb[prb], b16b], [lgTb], lambda e: e.activation(out=lgT[:, c0:c0 + W], in_=self.ps[prb][0:16, 0:W], func=AF.Identity, bias=b16[:, 0:1], scale=1.0))
                    pend = cur
                with cx.group("pe", [lgTb, self.cbuf], [self.psb[5]]) as box:
                    for tt in range(NT):
                        box.append(nc.tensor.transpose(out=self.ps[5][:, tt * 16:(tt + 1) * 16], in_=lgT[0:16, tt * 128:(tt + 1) * 128], identity=self.ident_f[0:16, 0:16]))
                NE4 = NT * 4
                sgm, bi, mb, eq, mb2, gates = R
                m1, m2, gs, gsel = Rs
                v3 = lambda t: t[:].rearrange("p t (g k) -> p (t g) k", k=4)
                f2 = lambda t: t[:].rearrange("p t e -> p (t e)")
                cx.op("act", [self.psb[5]], [Rb[0]], lambda e: e.activation(out=f2(sgm), in_=self.ps[5][:, 0:NT * 16], func=AF.Sigmoid))
                cx.op("dve", [Rb[0], rwb], [Rb[1]], lambda e: e.tensor_tensor(out=bi[:], in0=sgm[:], in1=rb[:].unsqueeze(1).to_broadcast([128, NT, 16]), op=ALU.add))
                cx.op("dve", [Rb[1]], [Rsb[0]], lambda e: e.tensor_reduce(out=m1[:], in_=v3(bi), axis=AX.X, op=ALU.max))
                cx.op("dve", [Rb[1], Rsb[0]], [Rb[3]], lambda e: e.tensor_tensor(out=v3(eq), in0=v3(bi), in1=m1[:].unsqueeze(2).to_broadcast([128, NE4, 4]), op=ALU.is_equal))
                cx.op("dve", [Rb[3], Rb[1]], [Rb[4]], lambda e: e.scalar_tensor_tensor(out=mb2[:], in0=eq[:], scalar=-10.0, in1=bi[:], op0=ALU.mult, op1=ALU.add))
                cx.op("dve", [Rb[4]], [Rsb[1]], lambda e: e.tensor_reduce(out=m2[:], in_=v3(mb2), axis=AX.X, op=ALU.max))
                cx.op("dve", [Rsb[0], Rsb[1]], [Rsb[2]], lambda e: e.tensor_tensor(out=gs[:], in0=m1[:], in1=m2[:], op=ALU.add))
                gs3 = gs[:].rearrange("p (t g) -> p t g", g=4)
                cx.op("dve", [Rsb[2]], [Rsb[1]], lambda e: e.tensor_reduce(out=m2[:, 0:NT], in_=gs3, axis=AX.X, op=ALU.max))
                cx.op("dve", [Rsb[2], Rsb[1]], [Rsb[3]], lambda e: e.tensor_tensor(out=gsel[:].rearrange("p (t g) -> p t g", g=4), in0=gs3,
                                                                                    in1=m2[:, 0:NT].unsqueeze(2).to_broadcast([128, NT, 4]), op=ALU.is_equal))
                cx.op("dve", [Rb[1], Rsb[3]], [Rb[2]], lambda e: e.scalar_tensor_tensor(out=v3(mb), in0=v3(bi), scalar=2.0,
                                                                                        in1=gsel[:].unsqueeze(2).to_broadcast([128, NE4, 4]), op0=ALU.add, op1=ALU.mult))
                cx.op("dve", [Rb[2]], [Rsb[0]], lambda e: e.tensor_reduce(out=m1[:, 0:NT], in_=mb[:], axis=AX.X, op=ALU.max))
                cx.op("dve", [Rb[2], Rsb[0]], [Rb[3]], lambda e: e.tensor_tensor(out=eq[:], in0=mb[:], in1=m1[:, 0:NT].unsqueeze(2).to_broadcast([128, NT, 16]), op=ALU.is_equal))
                cx.op("dve", [Rb[3], Rb[2]], [Rb[4]], lambda e: e.scalar_tensor_tensor(out=mb2[:], in0=eq[:], scalar=-10.0, in1=mb[:], op0=ALU.mult, op1=ALU.add))
                cx.op("dve", [Rb[4]], [Rsb[1]], lambda e: e.tensor_reduce(out=m2[:, 0:NT], in_=mb2[:], axis=AX.X, op=ALU.max))
                cx.op("dve", [Rb[4], Rsb[1]], [Rb[2]], lambda e: e.tensor_tensor(out=mb[:], in0=mb2[:], in1=m2[:, 0:NT].unsqueeze(2).to_broadcast([128, NT, 16]), op=ALU.is_equal))
                cx.op("dve", [Rb[2], Rb[3]], [Rb[3]], lambda e: e.tensor_tensor(out=eq[:], in0=eq[:], in1=mb[:], op=ALU.add))
                cx.op("dve", [Rb[3], Rb[0]], [Rb[2]], lambda e: e.tensor_tensor(out=mb[:], in0=eq[:], in1=sgm[:], op=ALU.mult))
                cx.op("dve", [Rb[2]], [Rsb[0]], lambda e: e.tensor_reduce(out=m1[:, 0:NT], in_=mb[:], axis=AX.X, op=ALU.add))
                cx.op("dve", [Rsb[0]], [Rsb[1]], lambda e: e.reciprocal(out=m2[:, 0:NT], in_=m1[:, 0:NT]))
                cx.op("dve", [Rb[2], Rsb[1]], [Rb[5]], lambda e: e.tensor_tensor(out=gates[:], in0=mb[:], in1=m2[:, 0:NT].unsqueeze(2).to_broadcast([128, NT, 16]), op=ALU.mult))
                if half == 0:
                    self.tap("sgm", sgm[:], [Rb[0]])
                    self.tap("gates", gates[:], [Rb[5]])
                    self.tap("bi", bi[:], [Rb[1]])
                    self.tap("gs", gs[:], [Rsb[2]])
                    self.tap("gsel", gsel[:], [Rsb[3]])
                for g4 in range(NT // 4):
                    with cx.group("pe", [Rb[5], self.cbuf], [self.psb[6]]) as box:
                        for q in range(4):
                            box.append(nc.tensor.transpose(out=self.ps[6][0:16, q * 128:(q + 1) * 128], in_=gates[:, g4 * 4 + q, :], identity=self.ident_f[:]))
                    cx.op("act", [self.psb[6]], [gTb], lambda e: e.copy(out=gT[:, g4 * 512:(g4 + 1) * 512], in_=self.ps[6][0:16, :]))
                it = 0
                dn_pending = [None]
                for ex in range(NE):
                    if ex + 1 < NE:
                        if dn_pending[0] is not None:
                            dn_pending[0]()
                            dn_pending[0] = None
                        load_w(ex + 1, half)
                    wi = (half * NE + ex) % 2
                    sel_e = selt[ex % 2]
                    selb = seltb[ex % 2]
                    cx.op("pool", [o16b], [selb], lambda e: e.affine_select(out=sel_e[:], in_=ones16[:], pattern=[[0, 128]], compare_op=ALU.is_equal,
                                                                          fill=0.0, base=-ex, channel_multiplier=1))
                    for blk in range(nb):
                        cols = slice(blk * TB, (blk + 1) * TB)
                        hb_i = it % 2
                        cx.op("pe", [selb, gTb], [self.psb[4]],
                              lambda e: e.matmul(self.ps[4][:], lhsT=sel_e[:], rhs=gT[:, cols], start=True, stop=True))
                        cx.op("act", [self.psb[4]], [Gsb], lambda e: e.copy(out=Gs[:], in_=self.ps[4][:]))
                        for dc in range(4):
                            pg = dc % 2
                            pu = 2 + dc % 2
                            with cx.group("pe", [wgb[wi], h2b[blk]], [self.psb[pg]]) as box:
                                for c in range(8):
                                    box.append(nc.tensor.matmul(self.ps[pg][:], lhsT=wg[wi][:, c, dc * 128:(dc + 1) * 128], rhs=h2[:, c, cols],
                                                                start=(c == 0), stop=(c == 7)))
                            with cx.group("pe", [wub[wi], h2b[blk]], [self.psb[pu]]) as box:
                                for c in range(8):
                                    box.append(nc.tensor.matmul(self.ps[pu][:], lhsT=wu[wi][:, c, dc * 128:(dc + 1) * 128], rhs=h2[:, c, cols],
                                                                start=(c == 0), stop=(c == 7)))
                            cx.op("act", [self.psb[pg]], [sgb], lambda e: e.activation(out=sg[:], in_=self.ps[pg][:], func=AF.Silu))
                            cx.op("dve", [sgb, Gsb], [t1b], lambda e: e.tensor_tensor(out=t1[:], in0=sg[:], in1=Gs[:], op=ALU.mult), big=True)
                            cx.op("dve", [t1b, self.psb[pu]], [heb[hb_i]], lambda e: e.tensor_tensor(out=he[hb_i][:, dc, :], in0=self.ps[pu][:], in1=t1[:], op=ALU.mult))
                        def down(wi=wi, hb_i=hb_i, blk=blk, cols=cols):
                            for fc in range(8):
                                pd = 5 + fc % 3
                                with cx.group("pe", [wdb[wi], heb[hb_i]], [self.psb[pd]]) as box:
                                    for dc in range(4):
                                        box.append(nc.tensor.matmul(self.ps[pd][:], lhsT=wd[wi][:, dc, fc * 128:(fc + 1) * 128], rhs=he[hb_i][:, dc, :],
                                                                    start=(dc == 0), stop=(dc == 3)))
                                cx.op("dve", [self.psb[pd], self.mod_buf[layer]], [xab[blk]],
                                      lambda e: e.scalar_tensor_tensor(out=xacc[:, fc, cols], in0=self.ps[pd][:], scalar=self.mod[:, layer, 40 + fc:41 + fc],
                                                                       in1=xacc[:, fc, cols], op0=ALU.mult, op1=ALU.add))
                        if dn_pending[0] is not None:
                            dn_pending[0]()
                        dn_pending[0] = down
                        it += 1
                if dn_pending[0] is not None:
                    dn_pending[0]()
                    dn_pending[0] = None
                for blk in range(nb):
                    gb = half * nb + blk
                    cx.dma("sp", self.xs[:, :, t0 + blk * TB: t0 + (blk + 1) * TB].rearrange("c p t -> p c t"),
                           xacc[:, :, blk * TB:(blk + 1) * TB], [xab[blk]], [self.xs_buf[gb]], self.xs_buf[gb])
            cx.barrier(self.xs_buf + xab + wgb + wub + wdb + [rwb])

    def phase_final(self):
        nc, cx = self.nc, self.cx
        with contextlib.ExitStack() as es:
            NB_ = 3
            xb_t = [self.sb(es, "fx%d" % i, [128, NCH, TB], F32) for i in range(NB_)]
            xbb = [Buf("fx%d" % i) for i in range(NB_)]
            tmp = [self.sb(es, "ftmp%d" % i, [128, NCH, TB], F32) for i in range(2)]
            tmpb = [Buf("ftmp%d" % i) for i in range(2)]
            sq = [self.sb(es, "fsq%d" % i, [128, NCH, TB], BF16) for i in range(2)]
            sqb = [Buf("fsq%d" % i) for i in range(2)]
            s_t = [self.sb(es, "fs%d" % i, [128, 2, TB], F32) for i in range(2)]
            sb_ = [Buf("fs%d" % i) for i in range(2)]
            ot = [self.sb(es, "fo%d" % i, [128, D], F32) for i in range(3)]
            otb = [Buf("fo%d" % i) for i in range(3)]

            def load(n):
                cx.dma("sp", xb_t[n % NB_][:], self.xs[:, :, n * TB:(n + 1) * TB].rearrange("c p t -> p c t"), [self.xs_buf[n]], [xbb[n % NB_]], xbb[n % NB_])

            def back(blk):
                i2 = blk % 2
                for tt in range(4):
                    n = blk * 4 + tt
                    o = ot[n % 3]
                    ob = otb[n % 3]
                    for half in range(2):
                        pb = (n * 2 + half) % 4
                        with cx.group("pe", [tmpb[i2], self.cbuf], [self.psb[pb]]) as box:
                            for q in range(4):
                                c = half * 4 + q
                                box.append(nc.tensor.transpose(out=self.ps[pb][:, q * 128:(q + 1) * 128],
                                                               in_=tmp[i2][:, c, tt * 128:(tt + 1) * 128], identity=self.ident_f[:]))
                        if half == 0:
                            cx.op("act", [self.psb[pb]], [ob], lambda e: e.copy(out=o[:, 0:512], in_=self.ps[pb][:]))
                        else:
                            cx.op("dve", [self.psb[pb]], [ob], lambda e: e.tensor_copy(out=o[:, 512:1024], in_=self.ps[pb][:]))
                    cx.dma("sp", self.out[n * 128:(n + 1) * 128, :], o[:], [ob], [self.out_buf], ob)

            for n in range(min(2, self.nblk)):
                load(n)
            for n in range(self.nblk + 1):
                if n < self.nblk:
                    if n + 2 < self.nblk:
                        load(n + 2)
                    i2 = n % 2
                    self.norm_block(TB, xb_t[n % NB_][:], xbb[n % NB_], self.gains[:, 4, :], None, self.gains_buf, None, None, sq[i2][:], sqb[i2],
                                    tmp[i2][:], tmpb[i2], s_t[i2], sb_[i2], 4 + i2, gain_only=True)
                if n > 0:
                    back(n - 1)
            cx.barrier(xbb + otb + [self.out_buf])
            for b_ in otb:
                if b_.dsem is not None:
                    nc.sync.wait_ge(b_.dsem.h, b_.dsem.val)

    def phase_gla(self):
        nc, cx = self.nc, self.cx
        A = self.AB[:, 0, 0, 0, :]
        B = self.AB[:, 0, 0, 1, :]
        ab = self.AB_buf[0][0]
        I = self.I
        with contextlib.ExitStack() as es:
            w_in = self.sb(es, "gwin", [128, 8, 3088], BF16)
            w_out = self.sb(es, "gwout", [128, 8, D], BF16)
            wb = Buf("gw")
            wg2 = self.sb(es, "gwg2", [16, 512], F32)
            bg2 = self.sb(es, "gbg2", [128, 4], F32)
            nbg2 = self.sb(es, "gnbg2", [128, 4], F32)
            gn = self.sb(es, "ggn", [128, 2], F32)
            lns = self.sb(es, "glns", [128, 1], F32)
            mask = self.sb(es, "gmask", [128, 64], F32)
            cmask = self.sb(es, "gcmask", [128, TB], F32)
            pb_ = Buf("gparams")
            for c in range(8):
                cx.dma("pool", w_in[:, c, :], I["gla_w_in"][c * 128:(c + 1) * 128, :], [], [wb], wb)
            cx.dma("pool", w_out[:], I["gla_w_out"].rearrange("(c p) n -> p c n", p=128), [], [wb], wb)
            cx.dma("sp", wg2[:], I["gla_w_gate2"], [], [pb_], pb_)
            cx.dma("sp", bg2[:], I["gla_b_gate2"], [], [pb_], pb_)
            cx.dma("sp", gn[:], I["gla_norm"], [], [pb_], pb_)
            cx.op("dve", [pb_], [pb_], lambda e: e.tensor_scalar(out=nbg2[:], in0=bg2[:], scalar1=-1.0, scalar2=None, op0=ALU.mult))
            cx.op("pool", [], [pb_], lambda e: e.memset(lns[:], float(np.log(128.0 ** -0.5))))
            cx.op("pool", [], [pb_], lambda e: e.memset(mask[:], 1.0))
            cx.op("pool", [pb_], [pb_], lambda e: e.affine_select(out=mask[0:64, :], in_=mask[0:64, :], pattern=[[1, 64]], compare_op=ALU.is_ge,
                                                                  fill=0.0, base=0, channel_multiplier=-1))
            cx.op("pool", [pb_], [pb_], lambda e: e.affine_select(out=mask[64:128, :], in_=mask[64:128, :], pattern=[[1, 64]], compare_op=ALU.is_ge,
                                                                  fill=0.0, base=self.mask_base, channel_multiplier=-1))
            cx.op("pool", [pb_], [pb_], lambda e: e.memset(cmask[:], 1.0))
            cx.op("pool", [pb_], [pb_], lambda e: e.memset(cmask[:].rearrange("p (c t) -> p c t", t=64)[:, :, 0:1], 0.0))
            self.tap("gmask", mask[:], [pb_])

            NW = 128
            xblk = self.sb(es, "gx", [128, NCH, TB], F32); xbb = Buf("gx")
            h = self.sb(es, "gh", [128, NCH, TB], BF16); hb = Buf("gh")
            tmpc = [self.sb(es, "gtmpc%d" % i_, [128, TB], F32) for i_ in range(2)]; tmpcb = [Buf("gtmpc%d" % i_) for i_ in range(2)]
            rs_ = self.sb(es, "grs", [128, TB], F32); sb_ = Buf("grs")
            glr = self.sb(es, "gglr", [16, TB], F32); glrb = Buf("gglr")
            e1 = self.sb(es, "ge1", [128, TB], F32); e1b = Buf("ge1")
            nla = self.sb(es, "gnla", [128, TB], F32); nlab = Buf("gnla")
            Bc = self.sb(es, "gBc", [128, TB], F32); Bcb = Buf("gBc")
            Eq = self.sb(es, "gEq", [128, TB], F32); Eqb = Buf("gEq")
            Ek = self.sb(es, "gEk", [128, TB], F32); Ekb = Buf("gEk")
            D2 = lambda nm, shp, dt: ([self.sb(es, nm + str(i_), shp, dt) for i_ in range(2)], [Buf(nm + str(i_)) for i_ in range(2)])
            dec_, decb_ = D2("gdec", [128, 4, 8], F32)
            qk_, qkb_ = D2("gqk", [128, 8, TB], BF16)
            v_, vb_ = D2("gv", [128, 4, D], BF16)
            sr_, srb_ = D2("gsr", [128, 8, TB], BF16)
            ktok_, ktb_ = D2("gktok", [128, 4, 512], BF16)
            Sm_, Smb_ = D2("gSm", [128, 16, 64], BF16)
            stf = self.sb(es, "gstf", [128, 4, 256], F32); stfb = [Buf("gstf%d" % i_) for i_ in range(2)]
            stb = self.sb(es, "gstb", [128, 4, 256], BF16); stbb = [Buf("gstb%d" % i_) for i_ in range(2)]
            sq2 = [self.sb(es, "gsq2%d" % i_, [128, 4, 256], BF16) for i_ in range(2)]; sq2b = [Buf("gsq2%d" % i_) for i_ in range(2)]
            s2 = [self.sb(es, "gs2%d" % i_, [128, TB], F32) for i_ in range(2)]; s2b = [Buf("gs2%d" % i_) for i_ in range(2)]
            t2 = [self.sb(es, "gt2%d" % i_, [128, 256], F32) for i_ in range(2)]; t2b = [Buf("gt2%d" % i_) for i_ in range(2)]
            og = self.sb(es, "gog", [128, 8, TB], BF16); ogb = Buf("gog")
            oraw = [self.sb(es, "goraw%d" % i_, [128, 4, 256], F32) for i_ in range(2)]; orawb = [Buf("goraw%d" % i_) for i_ in range(2)]
            for k2 in range(2):
                cx.op("pool", [], [stfb[k2]], lambda e: e.memset(stf[:, 2 * k2:2 * k2 + 2, :], 0.0))
                cx.op("pool", [], [stbb[k2]], lambda e: e.memset(stb[:, 2 * k2:2 * k2 + 2, :], 0.0))
            ps, psb = self.ps, self.psb
            rot = [0]

            def inproj(lhs_fn, rhs_fn, M=128):
                pb = rot[0] % 2
                rot[0] += 1
                with cx.group("pe", [wb, hb], [psb[pb]]) as box:
                    for c in range(8):
                        box.append(nc.tensor.matmul(ps[pb][0:M, :], lhsT=lhs_fn(c), rhs=rhs_fn(c), start=(c == 0), stop=(c == 7)))
                return pb

            def stageA(blk):
                si = blk % 2
                dec, decb, qk, qkb, v, vb = dec_[si], decb_[si], qk_[si], qkb_[si], v_[si], vb_[si]
                sr, srb, ktok, ktb, Sm, Smb = sr_[si], srb_[si], ktok_[si], ktb_[si], Sm_[si], Smb_[si]
                bcols = slice(blk * TB, (blk + 1) * TB)
                cx.dma("sp", xblk[:], self.xs[:, :, bcols].rearrange("c p t -> p c t"), [self.xs_buf[blk]], [xbb], xbb)
                for _ in range(5):
                    yield
                cx.op("act", [xbb], [hb], lambda e: e.activation(out=h[:], in_=xblk[:], func=AF.Square))
                yield
                yield
                pbn = rot[0] % 2
                rot[0] += 1
                with cx.group("pe", [hb, self.cbuf], [psb[pbn]]) as box:
                    for c in range(8):
                        box.append(nc.tensor.matmul(ps[pbn][:], lhsT=self.ones_b[:], rhs=h[:, c, :], start=(c == 0), stop=(c == 7)))
                cx.op("act", [psb[pbn]], [sb_], lambda e: e.activation(out=rs_[:], in_=ps[pbn][:], func=AF.Sqrt, bias=self.eps_t[:, 0:1], scale=1.0 / D))
                yield
                cx.op("dve", [sb_], [sb_], lambda e: e.reciprocal(out=rs_[:], in_=rs_[:]), big=True)
                yield
                for c in range(8):
                    ti = c % 2
                    cx.op("dve", [xbb, sb_, ab], [tmpcb[ti]], lambda e: e.scalar_tensor_tensor(out=tmpc[ti][:], in0=xblk[:, c, :], scalar=A[:, c:c + 1], in1=rs_[:], op0=ALU.mult, op1=ALU.mult))
                    cx.op("act", [tmpcb[ti], ab], [hb], lambda e: e.activation(out=h[:, c, :], in_=tmpc[ti][:], func=AF.Identity, bias=B[:, c:c + 1], scale=1.0))
                    if c % 2 == 1:
                        yield
                pb = inproj(lambda c: w_in[:, c, 3072:3088], lambda c: h[:, c, :], M=16)
                cx.op("act", [psb[pb]], [glrb], lambda e: e.copy(out=glr[:], in_=ps[pb][0:16, :]))
                yield
                for j in range(4):
                    pb = rot[0] % 2
                    rot[0] += 1
                    cx.op("pe", [pb_, glrb], [psb[pb]], lambda e: e.matmul(ps[pb][:], lhsT=wg2[:, j * 128:(j + 1) * 128], rhs=glr[:], start=True, stop=True))
                    cx.op("act", [psb[pb], pb_], [e1b], lambda e: e.activation(out=e1[:], in_=ps[pb][:], func=AF.Exp, bias=nbg2[:, j:j + 1], scale=-1.0))
                    cx.op("act", [e1b], [nlab], lambda e: e.activation(out=nla[:], in_=e1[:], func=AF.Ln, bias=1.0, scale=1.0))
                    cx.op("dve", [nlab, pb_], [Bcb], lambda e: e.tensor_tensor_scan(out=Bc[:], data0=cmask[:], data1=nla[:], initial=0.0, op0=ALU.mult, op1=ALU.add))
                    cx.op("act", [Bcb, pb_], [Eqb], lambda e: e.activation(out=Eq[:], in_=Bc[:], func=AF.Exp, bias=lns[:, 0:1], scale=-1.0 / 16.0))
                    cx.op("act", [Bcb], [Ekb], lambda e: e.activation(out=Ek[:], in_=Bc[:], func=AF.Exp, scale=1.0 / 16.0))
                    cx.op("act", [Bcb], [decb], lambda e: e.activation(out=dec[:, j, :], in_=Bc[:].rearrange("p (c t) -> p c t", t=64)[:, :, 63], func=AF.Exp, scale=-1.0 / 16.0))
                    yield
                    pb = inproj(lambda c: w_in[:, c, j * 128:(j + 1) * 128], lambda c: h[:, c, :])
                    cx.op("dve", [psb[pb], Eqb], [qkb], lambda e: e.tensor_tensor(out=qk[:, j, :], in0=ps[pb][:], in1=Eq[:], op=ALU.mult))
                    pb = inproj(lambda c: w_in[:, c, (4 + j) * 128:(5 + j) * 128], lambda c: h[:, c, :])
                    cx.op("dve", [psb[pb], Ekb], [qkb], lambda e: e.tensor_tensor(out=qk[:, 4 + j, :], in0=ps[pb][:], in1=Ek[:], op=ALU.mult))
                    yield
                for tt in range(4):
                    for hf in range(2):
                        pb = inproj(lambda c: h[:, c, tt * 128:(tt + 1) * 128], lambda c: w_in[:, c, 1024 + hf * 512: 1024 + (hf + 1) * 512])
                        cx.op("act", [psb[pb]], [vb], lambda e: e.copy(out=v[:, tt, hf * 512:(hf + 1) * 512], in_=ps[pb][:]))
                        yield
                for j in range(8):
                    pb = inproj(lambda c: w_in[:, c, 2048 + j * 128: 2048 + (j + 1) * 128], lambda c: h[:, c, :])
                    cx.op("act", [psb[pb]], [srb], lambda e: e.activation(out=sr[:, j, :], in_=ps[pb][:], func=AF.Silu))
                    yield
                for tt in range(4):
                    pbk = rot[0] % 2
                    rot[0] += 1
                    pvb = ps[pbk][:].bitcast(BF16)
                    with cx.group("pe", [qkb, self.cbuf], [psb[pbk]]) as box:
                        for hd in range(4):
                            box.append(nc.tensor.transpose(out=pvb[:, hd * 128:(hd + 1) * 128], in_=qk[:, 4 + hd, tt * 128:(tt + 1) * 128], identity=self.ident_b[:]))
                    cx.op("act", [psb[pbk]], [ktb], lambda e: e.copy(out=ktok[:, tt, :], in_=pvb[:, 0:512]))
                    yield
                for bank in range(2):
                    pbk = rot[0] % 2
                    rot[0] += 1
                    with cx.group("pe", [qkb], [psb[pbk]]) as box:
                        for t2_ in range(2):
                            tt = bank * 2 + t2_
                            for hd in range(4):
                                for hf in range(2):
                                    cc = slice(tt * 128 + hf * 64, tt * 128 + hf * 64 + 64)
                                    o0 = (t2_ * 4 + hd) * 64
                                    box.append(nc.tensor.matmul(ps[pbk][hf * 64:(hf + 1) * 64, o0:o0 + 64], lhsT=qk[:, 4 + hd, cc], rhs=qk[:, hd, cc], start=True, stop=True))
                    cx.op("dve", [psb[pbk], pb_], [Smb],
                          lambda e: e.tensor_tensor(out=Sm[:, bank * 8:(bank + 1) * 8, :], in0=ps[pbk][:].rearrange("p (a t) -> p a t", t=64),
                                                    in1=mask[:].unsqueeze(1).to_broadcast([128, 8, 64]), op=ALU.mult))
                    yield

            def pair_thread(blk, k):
                si = blk % 2
                dec, decb, qk, qkb, v, vb = dec_[si], decb_[si], qk_[si], qkb_[si], v_[si], vb_[si]
                sr, srb, ktok, ktb, Sm, Smb = sr_[si], srb_[si], ktok_[si], ktb_[si], Sm_[si], Smb_[si]
                bO = [2 + 3 * k, 3 + 3 * k]
                bP = 4 + 3 * k
                pO = [ps[b_][:].rearrange("p (j t) -> p j t", t=256) for b_ in bO]
                pP = ps[bP][:].rearrange("p (h v) -> p h v", v=256)
                stf_p = stf[:, 2 * k:2 * k + 2, :]
                stb_p = stb[:, 2 * k:2 * k + 2, :]
                post_steps = []
                for q in range(2):
                    for c4 in range(4):
                        c = q * 4 + c4
                        tt, hf = c // 2, c % 2
                        rows = slice(hf * 64, (hf + 1) * 64)
                        cc = slice(c * 64, (c + 1) * 64)
                        wc = slice(c4 * 64, (c4 + 1) * 64)
                        with cx.group("pe", [stbb[k], qkb, vb, Smb, ktb], [psb[bO[0]], psb[bO[1]], psb[bP]]) as box:
                            for hd2 in range(2):
                                hd = 2 * k + hd2
                                for j in range(2):
                                    box.append(nc.tensor.matmul(pO[hd2][:, j, wc], lhsT=stb[:, hd, j * 128:(j + 1) * 128], rhs=qk[:, hd, cc], start=True, stop=False))
                                    box.append(nc.tensor.matmul(pO[hd2][:, j, wc], lhsT=v[rows, tt, hd * 256 + j * 128: hd * 256 + (j + 1) * 128],
                                                                rhs=Sm[rows, tt * 4 + hd, :], start=False, stop=True))
                                box.append(nc.tensor.matmul(pP[:, hd2, :], lhsT=ktok[rows, tt, hd * 128:(hd + 1) * 128],
                                                            rhs=v[rows, tt, hd * 256:(hd + 1) * 256], start=True, stop=True))
                        yield
                        cx.op("dve", [psb[bP]], [stfb[k]], lambda e: e.tensor_tensor(out=stf_p, in0=pP, in1=stf_p, op=ALU.add), big=True)
                        cx.op("dve", [decb], [stfb[k]], lambda e: e.tensor_tensor(out=stf_p, in0=stf_p, in1=dec[:, 2 * k:2 * k + 2, c:c + 1].to_broadcast([128, 2, 256]), op=ALU.mult), big=True)
                        yield
                        cx.op("act", [stfb[k]], [stbb[k]], lambda e: e.copy(out=stb_p, in_=stf_p))
                        if post_steps:
                            post_steps.pop(0)()
                        yield
                    while post_steps:
                        post_steps.pop(0)()
                    qc = slice(q * 256, (q + 1) * 256)
                    for hd2 in range(2):
                        cx.op("act", [psb[bO[hd2]]], [sq2b[k]], lambda e: e.activation(out=sq2[k][:, hd2 * 2:hd2 * 2 + 2, :], in_=pO[hd2], func=AF.Square))
                        cx.op("act", [psb[bO[hd2]]], [orawb[k]], lambda e: e.copy(out=oraw[k][:, hd2 * 2:hd2 * 2 + 2, :], in_=pO[hd2]))
                    yield

                    def p1(qc=qc):
                        pbs = rot[0] % 2
                        rot[0] += 1
                        with cx.group("pe", [sq2b[k], self.cbuf], [psb[pbs]]) as box:
                            for hd2 in range(2):
                                for j in range(2):
                                    box.append(nc.tensor.matmul(ps[pbs][:, hd2 * 256:(hd2 + 1) * 256], lhsT=self.ones_b[:], rhs=sq2[k][:, hd2 * 2 + j, :], start=(j == 0), stop=(j == 1)))
                        cx.op("act", [psb[pbs]], [s2b[k]], lambda e: e.activation(out=s2[k][:], in_=ps[pbs][:], func=AF.Sqrt, bias=self.eps_t[:, 0:1], scale=1.0 / 256.0))
                        cx.op("dve", [s2b[k]], [s2b[k]], lambda e: e.reciprocal(out=s2[k][:], in_=s2[k][:]), big=True)

                    def p2(hd2, qc=qc):
                        hd = 2 * k + hd2
                        for j in range(2):
                            cx.op("dve", [orawb[k], s2b[k], pb_], [t2b[k]], lambda e: e.scalar_tensor_tensor(out=t2[k][:], in0=oraw[k][:, hd2 * 2 + j, :], scalar=gn[:, j:j + 1],
                                                                                                            in1=s2[k][:, hd2 * 256:(hd2 + 1) * 256], op0=ALU.mult, op1=ALU.mult), big=True)
                            cx.op("dve", [t2b[k], srb], [ogb], lambda e: e.tensor_tensor(out=og[:, hd * 2 + j, qc], in0=t2[k][:], in1=sr[:, hd * 2 + j, qc], op=ALU.mult))

                    post_steps.extend([p1, lambda: p2(0), lambda: p2(1)])
                while post_steps:
                    post_steps.pop(0)()
                    yield

            def stageC(blk):
                bcols = slice(blk * TB, (blk + 1) * TB)
                cx.dma("sp", xblk[:], self.xs[:, :, bcols].rearrange("c p t -> p c t"), [self.xs_buf[blk]], [xbb], xbb)
                for fc in range(8):
                    pb = rot[0] % 2
                    rot[0] += 1
                    with cx.group("pe", [wb, ogb], [psb[pb]]) as box:
                        for c in range(8):
                            box.append(nc.tensor.matmul(ps[pb][:], lhsT=w_out[:, c, fc * 128:(fc + 1) * 128], rhs=og[:, c, :], start=(c == 0), stop=(c == 7)))
                    cx.op("dve", [psb[pb], self.mod_buf[0]], [xbb], lambda e: e.scalar_tensor_tensor(out=xblk[:, fc, :], in0=ps[pb][:], scalar=self.mod[:, 0, 16 + fc:17 + fc],
                                                                                                    in1=xblk[:, fc, :], op0=ALU.mult, op1=ALU.add))
                cx.dma("sp", self.xs[:, :, bcols].rearrange("c p t -> p c t"), xblk[:], [xbb], [self.xs_buf[blk]], self.xs_buf[blk])

            run_threads([stageA(0)])
            for blk in range(self.nblk):
                if blk > 0:
                    stageC(blk - 1)
                th = [pair_thread(blk, 0), pair_thread(blk, 1)]
                wts = [1, 1]
                if blk + 1 < self.nblk:
                    th.append(stageA(blk + 1))
                    wts.append(2)
                run_threads(th, wts)
            stageC(self.nblk - 1)
            cx.barrier(self.xs_buf + [xbb, wb, pb_])

    def phase_ssd(self):
        nc = self.nc
        NTL = self.ntok // 128
        self.xcs = nc.dram_tensor("xcs", [NTL, 128, 24, 128], BF16).ap()
        self.zs = nc.dram_tensor("zs", [NTL, 128, 2048], BF16).ap()
        self.dts = nc.dram_tensor("dts", [NTL, 32, 2, 128], F32).ap()
        self.scr = Buf("ssd_scr")
        self.phase_ssd_a()
        self.phase_ssd_b()

    def phase_ssd_a(self):
        nc, cx = self.nc, self.cx
        A = self.AB[:, 1, 0, 0, :]
        B = self.AB[:, 1, 0, 1, :]
        ab = self.AB_buf[1][0]
        I = self.I
        W = 256
        ntok = self.ntok
        scr = self.scr
        with contextlib.ExitStack() as es:
            hall = self.sb(es, "sha", [128, NCH, ntok], BF16); hab = Buf("sha")
            xsub = [self.sb(es, "sxs%d" % i, [128, NCH, W], F32) for i in range(4)]; xsubb = [Buf("sxs%d" % i) for i in range(4)]
            tmp = [self.sb(es, "stmp%d" % i, [128, NCH, W], F32) for i in range(4)]; tmpb = [Buf("stmp%d" % i) for i in range(4)]
            sq = [self.sb(es, "ssq%d" % i, [128, NCH, W], BF16) for i in range(4)]; sqb = [Buf("ssq%d" % i) for i in range(4)]
            s_t = [self.sb(es, "ss_t%d" % i, [128, 2, W], F32) for i in range(4)]; sb_ = [Buf("ss_t%d" % i) for i in range(4)]
            wgrp = [self.sb(es, "swg%d" % i, [128, 8, 512], BF16) for i in range(3)]
            wgb = [Buf("swg%d" % i) for i in range(3)]
            cw = self.sb(es, "scw", [128, 24, 4], F32)
            cbias = self.sb(es, "scb", [128, 24], F32)
            dtb = self.sb(es, "sdtb", [32, 1], F32)
            alog = self.sb(es, "salog", [32, 1], F32)
            aneg = self.sb(es, "saneg", [32, 1], F32)
            hist = self.sb(es, "shist", [128, 3], BF16); histb = Buf("shist")
            pb_ = Buf("sparams")
            cx.dma("sp", cw[:], I["ssd_conv_w"], [], [pb_], pb_)
            cx.dma("sp", cbias[:], I["ssd_conv_b"], [], [pb_], pb_)
            cx.dma("sp", dtb[:], I["ssd_dt_bias"], [], [pb_], pb_)
            cx.dma("sp", alog[:], I["ssd_a_log"], [], [pb_], pb_)
            cx.op("act", [pb_], [pb_], lambda e: e.activation(out=aneg[:], in_=alog[:], func=AF.Exp))
            cx.op("dve", [pb_], [pb_], lambda e: e.tensor_scalar(out=aneg[:], in0=aneg[:], scalar1=-1.0, scalar2=None, op0=ALU.mult))
            u = [self.sb(es, "su%d" % i, [128, 515], BF16) for i in range(2)]; ub = [Buf("su%d" % i) for i in range(2)]
            acc = [self.sb(es, "sacc%d" % i, [128, TB], F32) for i in range(2)]; accb = [Buf("sacc%d" % i) for i in range(2)]
            xcq = [self.sb(es, "sxcq%d" % i, [128, TB], BF16) for i in range(3)]; xcqb = [Buf("sxcq%d" % i) for i in range(3)]
            zt = [self.sb(es, "szt%d" % i, [128, TB], BF16) for i in range(3)]; ztb = [Buf("szt%d" % i) for i in range(3)]
            e1 = self.sb(es, "se1", [32, TB], F32); e1b = Buf("se1")
            dtT = [self.sb(es, "sdtT%d" % i, [32, 2, TB], F32) for i in range(2)]; dtTb = [Buf("sdtT%d" % i) for i in range(2)]
            ps, psb = self.ps, self.psb
            rot = [0]
            wrot = [0]

            def load_grp(c0, ncols):
                i = wrot[0] % 3
                wrot[0] += 1
                cx.dma("pool", wgrp[i][:, :, 0:ncols], I["ssd_w_in"][:, c0:c0 + ncols].rearrange("(c p) n -> p c n", p=128), [], [wgb[i]], wgb[i])
                return i

            nxt = load_grp(0, 512)
            n = 0
            gens_ = []
            nsub = self.nblk * (TB // W)

            def issue_load(n):
                blk, sub = divmod(n, TB // W)
                c0 = blk * TB + sub * W
                i2 = n % 4
                cx.dma("sp", xsub[i2][:], self.xs[:, :, c0:c0 + W].rearrange("c p t -> p c t"), [self.xs_buf[blk]], [xsubb[i2]], xsubb[i2])

            for n in range(min(3, nsub)):
                issue_load(n)
            pend = None
            for n in range(nsub + 1):
                cur = None
                if n < nsub:
                    blk, sub = divmod(n, TB // W)
                    c0 = blk * TB + sub * W
                    i2 = n % 4
                    if n + 3 < nsub:
                        issue_load(n + 3)
                    g_ = self.norm_block_gen(W, xsub[i2][:], xsubb[i2], A, B, ab, hall[:, :, c0:c0 + W], hab, sq[i2][:], sqb[i2], tmp[i2][:], tmpb[i2], s_t[i2], sb_[i2], 2 + i2)
                    for _ in range(3):
                        next(g_)
                    cur = g_
                if pend is not None:
                    for _ in pend:
                        pass
                pend = cur
            nz = 0
            for zg in range(4):
                wi = nxt
                nxt = load_grp((zg + 1) * 512, 512)
                for blk in range(self.nblk):
                    for tt in range(4):
                        tile_ = blk * 4 + tt
                        pb = rot[0] % 2
                        rot[0] += 1
                        with cx.group("pe", [wgb[wi], hab], [psb[pb]]) as box:
                            for c in range(8):
                                box.append(nc.tensor.matmul(ps[pb][:], lhsT=hall[:, c, tile_ * 128:(tile_ + 1) * 128], rhs=wgrp[wi][:, c, :], start=(c == 0), stop=(c == 7)))
                        zi = nz % 3
                        nz += 1
                        cx.op("act", [psb[pb]], [ztb[zi]], lambda e: e.activation(out=zt[zi][:], in_=ps[pb][:], func=AF.Silu))
                        cx.dma("sp", self.zs[tile_, :, zg * 512:(zg + 1) * 512], zt[zi][:], [ztb[zi]], [scr], ztb[zi])
            nq = 0
            deferred = [None]
            for xg in range(6):
                wi = nxt
                nxt = load_grp(2048 + (xg + 1) * 512, 512) if xg < 5 else load_grp(5120, 32)
                for q in range(4):
                    ch = xg * 4 + q
                    cx.op("dve", [], [histb], lambda e: e.memset(hist[:], 0.0))
                    for blk in range(self.nblk):
                        bcols = slice(blk * TB, (blk + 1) * TB)
                        pb = rot[0] % 2
                        rot[0] += 1
                        ui = nq % 2
                        xi = nq % 3
                        nq += 1
                        with cx.group("pe", [wgb[wi], hab], [psb[pb]]) as box:
                            for c in range(8):
                                box.append(nc.tensor.matmul(ps[pb][:], lhsT=wgrp[wi][:, c, q * 128:(q + 1) * 128], rhs=hall[:, c, bcols], start=(c == 0), stop=(c == 7)))
                        cx.op("act", [histb], [ub[ui]], lambda e: e.copy(out=u[ui][:, 0:3], in_=hist[:]))
                        cx.op("act", [psb[pb]], [ub[ui]], lambda e: e.copy(out=u[ui][:, 3:515], in_=ps[pb][:]))
                        cx.op("act", [ub[ui]], [histb], lambda e: e.copy(out=hist[:], in_=u[ui][:, 512:515]))
                        cx.op("act", [psb[pb], pb_], [accb[ui]], lambda e: e.activation(out=acc[ui][:], in_=ps[pb][:], func=AF.Identity, scale=cw[:, ch, 3:4]))
                        for j in range(3):
                            cx.op("dve", [ub[ui], pb_], [accb[ui]], lambda e: e.scalar_tensor_tensor(out=acc[ui][:], in0=u[ui][:, j:j + 512], scalar=cw[:, ch, j:j + 1],
                                                                                                     in1=acc[ui][:], op0=ALU.mult, op1=ALU.add), big=True)
                        def fin(ui=ui, xi=xi, ch=ch, blk=blk):
                            cx.op("act", [accb[ui], pb_], [xcqb[xi]], lambda e: e.activation(out=xcq[xi][:], in_=acc[ui][:], func=AF.Silu, bias=cbias[:, ch:ch + 1], scale=1.0))
                            cx.dma("sp", self.xcs[blk * 4:(blk + 1) * 4, :, ch, :].rearrange("t p k -> p t k"), xcq[xi][:].rearrange("p (t k) -> p t k", k=128),
                                   [xcqb[xi]], [scr], xcqb[xi])
                        if deferred[0] is not None:
                            deferred[0]()
                        deferred[0] = fin
            if deferred[0] is not None:
                deferred[0]()
            wi = nxt
            for blk in range(self.nblk):
                bcols = slice(blk * TB, (blk + 1) * TB)
                pb = rot[0] % 2
                rot[0] += 1
                di = blk % 2
                with cx.group("pe", [wgb[wi], hab], [psb[pb]]) as box:
                    for c in range(8):
                        box.append(nc.tensor.matmul(ps[pb][0:32, :], lhsT=wgrp[wi][:, c, 0:32], rhs=hall[:, c, bcols], start=(c == 0), stop=(c == 7)))
                cx.op("act", [psb[pb], pb_], [e1b], lambda e: e.activation(out=e1[:], in_=ps[pb][0:32, :], func=AF.Exp, bias=dtb[:, 0:1], scale=1.0))
                cx.op("act", [e1b], [dtTb[di]], lambda e: e.activation(out=dtT[di][:, 0, :], in_=e1[:], func=AF.Ln, bias=1.0, scale=1.0))
                cx.op("dve", [dtTb[di], pb_], [dtTb[di]], lambda e: e.tensor_scalar(out=dtT[di][:, 1, :], in0=dtT[di][:, 0, :], scalar1=aneg[:, 0:1], scalar2=None, op0=ALU.mult))
                for a_ in range(2):
                    cx.dma("sp", self.dts[blk * 4:(blk + 1) * 4, :, a_, :].rearrange("t h k -> h t k"), dtT[di][:, a_, :].rearrange("h (t k) -> h t k", k=128),
                           [dtTb[di]], [scr], dtTb[di])
            cx.barrier([scr, pb_] + ztb + xcqb + dtTb + wgb + xsubb + self.xs_buf)

    def phase_ssd_b(self):
        nc, cx = self.nc, self.cx
        I = self.I
        NTL = self.ntok // 128
        scr = self.scr
        with contextlib.ExitStack() as es:
            w_out = self.sb(es, "swout", [128, 16, D], BF16)
            wob = Buf("swout")
            cx.dma("pool", w_out[:], I["ssd_w_out"].rearrange("(c p) n -> p c n", p=128), [], [wob], wob)
            dbc = self.sb(es, "sdbc", [128, 32], F32)
            ngcol = self.sb(es, "sngcol", [128, 16], F32)
            mask = self.sb(es, "smask", [128, 64], F32)
            imask = self.sb(es, "simask", [128, 64], F32)
            ones2 = self.sb(es, "sones2", [128, 128], F32)
            onesh = self.sb(es, "sonesh", [128, 2, 128], F32)
            tri2 = self.sb(es, "stri2", [128, 128], F32)
            Dsk = self.sb(es, "sDsk", [128, 32, 64], BF16)
            pb_ = Buf("sparams")
            cx.dma("sp", dbc[:], I["ssd_d_bc"], [], [pb_], pb_)
            cx.dma("sp", ngcol[:], I["ssd_norm_col"], [], [pb_], pb_)
            for c in range(16):
                cx.op("dve", [pb_, wob], [wob], lambda e: e.tensor_scalar(out=w_out[:, c, :], in0=w_out[:, c, :], scalar1=ngcol[:, c:c + 1], scalar2=None, op0=ALU.mult))
            P_ = lambda fn: cx.op("pool", [pb_], [pb_], fn)
            P_(lambda e: e.memset(mask[:], 1.0))
            for hf in range(2):
                r_ = slice(hf * 64, (hf + 1) * 64)
                P_(lambda e: e.affine_select(out=mask[r_, :], in_=mask[r_, :], pattern=[[1, 64]], compare_op=ALU.is_ge, fill=0.0, base=0, channel_multiplier=-1))
            P_(lambda e: e.memset(imask[:], 1.0))
            for hf in range(2):
                r_ = slice(hf * 64, (hf + 1) * 64)
                P_(lambda e: e.affine_select(out=imask[r_, :], in_=imask[r_, :], pattern=[[1, 64]], compare_op=ALU.is_equal, fill=0.0, base=0, channel_multiplier=-1))
            P_(lambda e: e.memset(ones2[:], 0.0))
            P_(lambda e: e.memset(onesh[:], 0.0))
            P_(lambda e: e.memset(tri2[:], 0.0))
            for hf in range(2):
                r_ = slice(hf * 64, (hf + 1) * 64)
                P_(lambda e: e.memset(ones2[r_, hf * 64:(hf + 1) * 64], 1.0))
                P_(lambda e: e.memset(onesh[r_, hf, :], 1.0))
                P_(lambda e: e.tensor_copy(out=tri2[r_, hf * 64:(hf + 1) * 64], in_=mask[r_, :]))
            cx.op("dve", [pb_], [pb_], lambda e: e.tensor_tensor(out=Dsk[:], in0=dbc[:].unsqueeze(2).to_broadcast([128, 32, 64]),
                                                                 in1=imask[:].unsqueeze(1).to_broadcast([128, 32, 64]), op=ALU.mult))
            L3 = lambda nm, shp, dt: ([self.sb(es, nm + str(i), shp, dt) for i in range(3)], [Buf(nm + str(i)) for i in range(3)])
            L2 = lambda nm, shp, dt: ([self.sb(es, nm + str(i), shp, dt) for i in range(2)], [Buf(nm + str(i)) for i in range(2)])
            L4 = lambda nm, shp, dt: ([self.sb(es, nm + str(i), shp, dt) for i in range(4)], [Buf(nm + str(i)) for i in range(4)])
            xct, xctb = L3("sxct", [128, 24, 128], BF16)
            sztt, szttb = L3("sszt", [128, 2048], BF16)
            dtt, dttb = L3("sdtt", [32, 2, 128], F32)
            xch, xchb = L4("sxch", [128, TB], F32)
            xtok, xtokb = L2("sxtok", [128, 2048], BF16)
            Btok, Btokb = L2("sBtok", [128, 512], BF16)
            dd, ddb = L2("sdd", [128, 2, 32], F32)
            sm, smb = L2("ssm", [128, 8, 32], F32)
            CBm, CBmb = L2("sCBm", [128, 4, 64], F32)
            Dm, Dmb = L2("sDm", [128, 8, 64], F32)
            dif, difb = L2("sdif", [128, 8, 64], F32)
            Mt4, _ = L2("sMt", [128, 32, 64], BF16)
            xdt4, _ = L2("sxdt", [128, 2048], BF16)
            xw4, _ = L2("sxw", [128, 2048], BF16)
            Mt4b = [[Buf("sMt%d_%d" % (i_, g_)) for g_ in range(4)] for i_ in range(2)]
            xdt4b = [[Buf("sxdt%d_%d" % (i_, g_)) for g_ in range(4)] for i_ in range(2)]
            xw4b = [[Buf("sxw%d_%d" % (i_, g_)) for g_ in range(4)] for i_ in range(2)]
            ty, tyb = L4("sty", [128, 512], F32)
            yz, yzb = L4("syz", [128, 512], F32)
            y1s, y1sb = L4("sy1s", [128, 512], F32)
            jk, jkb = L2("sjk", [128, 512], BF16)
            jk = jk + jk; jkb = jkb + jkb
            ssq, ssqb = L4("sssq", [128, 2], F32)
            yn, ynb = L2("syn", [128, 2048], BF16)
            ynT, ynTb = L2("synT", [128, 16, TB], BF16)
            stf = self.sb(es, "sstf", [128, 4, 512], F32); stfb = [Buf("sstf%d" % i) for i in range(4)]
            stb = self.sb(es, "sstb", [128, 4, 512], BF16); stbb = [Buf("sstb%d" % i) for i in range(4)]
            for g in range(4):
                cx.op("pool", [], [stfb[g]], lambda e: e.memset(stf[:, g, :], 0.0))
                cx.op("pool", [], [stbb[g]], lambda e: e.memset(stb[:, g, :], 0.0))
            ps, psb = self.ps, self.psb
            rot = [0]

            def load(t):
                i3 = t % 3
                cx.dma("sp", xct[i3][:], self.xcs[t], [scr], [xctb[i3]], xctb[i3])
                cx.dma("sp", sztt[i3][:], self.zs[t], [scr], [szttb[i3]], szttb[i3])
                cx.dma("sp", dtt[i3][:], self.dts[t], [scr], [dttb[i3]], dttb[i3])

            def pre(t):
                i2 = t % 2
                i3 = t % 3
                xc_ = xct[i3]
                xcb = xctb[i3]
                for hbk in range(2):
                    pbk = rot[0] % 3
                    rot[0] += 1
                    pv = ps[pbk][:].bitcast(BF16)
                    with cx.group("pe", [xcb, self.cbuf], [psb[pbk]]) as box:
                        for q in range(8):
                            box.append(nc.tensor.transpose(out=pv[:, q * 128:(q + 1) * 128], in_=xc_[:, hbk * 8 + q, :], identity=self.ident_b[:]))
                    cx.op("act", [psb[pbk]], [xtokb[i2]], lambda e: e.copy(out=xtok[i2][:, hbk * 1024:(hbk + 1) * 1024], in_=pv[:, 0:1024]))
                    yield
                pbk = rot[0] % 3
                rot[0] += 1
                pv5 = ps[pbk][:].bitcast(BF16)
                with cx.group("pe", [xcb, self.cbuf], [psb[pbk]]) as box:
                    for g in range(4):
                        box.append(nc.tensor.transpose(out=pv5[:, g * 128:(g + 1) * 128], in_=xc_[:, 16 + g, :], identity=self.ident_b[:]))
                cx.op("act", [psb[pbk]], [Btokb[i2]], lambda e: e.copy(out=Btok[i2][:], in_=pv5[:, 0:512]))
                yield
                p4 = ps[3]
                b4 = psb[3]
                with cx.group("pe", [dttb[i3], self.cbuf], [b4]) as box:
                    for k2 in range(2):
                        box.append(nc.tensor.transpose(out=p4[:, k2 * 32:(k2 + 1) * 32], in_=dtt[i3][:, k2, :], identity=self.ident_f[0:32, 0:32]))
                yield
                cx.op("act", [b4], [ddb[i2]], lambda e: e.copy(out=dd[i2][:].rearrange("p a h -> p (a h)"), in_=p4[:, 0:64]))
                yield
                with cx.group("pe", [ddb[i2], pb_, xcb], [b4]) as box:
                    box.append(nc.tensor.matmul(p4[:, 0:32], lhsT=tri2[:], rhs=dd[i2][:, 1, :], start=True, stop=True))
                    box.append(nc.tensor.matmul(p4[:, 32:64], lhsT=ones2[:], rhs=dd[i2][:, 1, :], start=True, stop=True))
                    for hf in range(2):
                        box.append(nc.tensor.matmul(p4[:, 64 + hf * 32: 96 + hf * 32], lhsT=onesh[:, hf, :], rhs=dd[i2][:, 1, :], start=True, stop=True))
                    for g in range(4):
                        for hf in range(2):
                            cc = slice(hf * 64, hf * 64 + 64)
                            box.append(nc.tensor.matmul(p4[hf * 64:(hf + 1) * 64, 128 + g * 64: 192 + g * 64], lhsT=xc_[:, 16 + g, cc], rhs=xc_[:, 20 + g, cc], start=True, stop=True))
                yield
                sm_ = sm[i2]
                sb2 = smb[i2]
                cx.op("act", [b4], [sb2], lambda e: e.copy(out=sm_[:, 0, :], in_=p4[:, 0:32]))
                cx.op("act", [b4], [sb2], lambda e: e.activation(out=sm_[:, 1, :], in_=p4[:, 0:32], func=AF.Exp))
                cx.op("act", [b4], [sb2], lambda e: e.activation(out=sm_[:, 4:6, :].rearrange("p a h -> p (a h)"), in_=p4[:, 64:128], func=AF.Exp))
                cx.op("dve", [b4, sb2], [sb2], lambda e: e.tensor_tensor(out=sm_[:, 6, :], in0=p4[:, 32:64], in1=sm_[:, 0, :], op=ALU.subtract))
                cx.op("act", [sb2], [sb2], lambda e: e.activation(out=sm_[:, 2, :], in_=sm_[:, 6, :], func=AF.Exp))
                cx.op("dve", [sb2, ddb[i2]], [sb2], lambda e: e.tensor_tensor(out=sm_[:, 3, :], in0=sm_[:, 2, :], in1=dd[i2][:, 0, :], op=ALU.mult))
                cx.op("dve", [b4, pb_], [CBmb[i2]], lambda e: e.tensor_tensor(out=CBm[i2][:], in0=p4[:, 128:384].rearrange("p (g t) -> p g t", t=64),
                                                                             in1=mask[:].unsqueeze(1).to_broadcast([128, 4, 64]), op=ALU.mult))
                yield
                v3 = lambda ap: ap.rearrange("p (h q) -> p h q", q=64)
                for g in range(4):
                    k = g % 2
                    hs = slice(g * 8, (g + 1) * 8)
                    gcols = slice(g * 512, (g + 1) * 512)
                    cx.op("pool", [ddb[i2], pb_], [Dmb[k]], lambda e: e.tensor_tensor(out=Dm[k][:], in0=dd[i2][:, 1, hs].unsqueeze(2).to_broadcast([128, 8, 64]),
                                                                                     in1=mask[:].unsqueeze(1).to_broadcast([128, 8, 64]), op=ALU.mult))
                    cx.op("pool", [xtokb[i2], ddb[i2]], [xdt4b[i2][g]], lambda e: e.tensor_tensor(out=v3(xdt4[i2][:, gcols]), in0=v3(xtok[i2][:, gcols]), in1=dd[i2][:, 0, hs].unsqueeze(2).to_broadcast([128, 8, 64]), op=ALU.mult))
                    cx.op("pool", [xtokb[i2], sb2], [xw4b[i2][g]], lambda e: e.tensor_tensor(out=v3(xw4[i2][:, gcols]), in0=v3(xtok[i2][:, gcols]), in1=sm_[:, 3, hs].unsqueeze(2).to_broadcast([128, 8, 64]), op=ALU.mult))
                    yield
                    pbk = rot[0] % 3
                    rot[0] += 1
                    cx.op("pe", [Dmb[k], pb_], [psb[pbk]], lambda e: e.matmul(ps[pbk][:], lhsT=ones2[:], rhs=Dm[k][:].rearrange("p h t -> p (h t)"), start=True, stop=True))
                    cx.op("dve", [psb[pbk], sb2], [difb[k]], lambda e: e.tensor_tensor(out=dif[k][:], in0=v3(ps[pbk][:]), in1=sm_[:, 0, hs].unsqueeze(2).to_broadcast([128, 8, 64]), op=ALU.subtract), big=True)
                    yield
                    cx.op("act", [difb[k]], [difb[k]], lambda e: e.activation(out=dif[k][:], in_=dif[k][:], func=AF.Exp))
                    yield
                    cx.op("dve", [difb[k], CBmb[i2]], [Mt4b[i2][g]], lambda e: e.scalar_tensor_tensor(out=Mt4[i2][:, hs, :], in0=dif[k][:], scalar=1.0, in1=CBm[i2][:, g, :].unsqueeze(1).to_broadcast([128, 8, 64]),
                                                                                                  op0=ALU.min, op1=ALU.mult))
                    yield

            def grp(t, g, k):
                i2 = t % 2
                i3 = t % 3
                xc_ = xct[i3]
                xcb = xctb[i3]
                bk = 4 + g
                hs = slice(g * 8, (g + 1) * 8)
                gcols = slice(g * 512, (g + 1) * 512)
                sm_ = sm[i2]
                v3 = lambda ap: ap.rearrange("p (h q) -> p h q", q=64)
                with cx.group("pe", [Mt4b[i2][g], xdt4b[i2][g], xtokb[i2], pb_], [psb[bk]]) as box:
                    for hh in range(8):
                        for hf in range(2):
                            rows = slice(hf * 64, (hf + 1) * 64)
                            o_ = ps[bk][rows, hh * 64:(hh + 1) * 64]
                            box.append(nc.tensor.matmul(o_, lhsT=Mt4[i2][rows, g * 8 + hh, :], rhs=xdt4[i2][rows, g * 512 + hh * 64: g * 512 + (hh + 1) * 64], start=True, stop=False))
                            box.append(nc.tensor.matmul(o_, lhsT=Dsk[rows, g * 8 + hh, :], rhs=xtok[i2][rows, g * 512 + hh * 64: g * 512 + (hh + 1) * 64], start=False, stop=True))
                cx.op("act", [psb[bk]], [y1sb[k]], lambda e: e.copy(out=y1s[k][:], in_=ps[bk][:]))
                yield
                for hf in range(2):
                    rows = slice(hf * 64, (hf + 1) * 64)
                    cc = slice(hf * 64, hf * 64 + 64)
                    cx.op("pe", [xcb, stbb[g]], [psb[bk]], lambda e: e.matmul(ps[bk][rows, :], lhsT=xc_[:, 20 + g, cc], rhs=stb[:, g, :], start=True, stop=True))
                    cx.op("dve", [psb[bk], smb[i2]], [tyb[k]], lambda e: e.tensor_tensor(out=v3(ty[k][rows, :]), in0=v3(ps[bk][rows, :]), in1=sm_[rows, 1, hs].unsqueeze(2).to_broadcast([64, 8, 64]), op=ALU.mult), big=True)
                    yield
                    cx.op("pe", [Btokb[i2], xw4b[i2][g]], [psb[bk]], lambda e: e.matmul(ps[bk][:], lhsT=Btok[i2][rows, g * 128:(g + 1) * 128], rhs=xw4[i2][rows, gcols], start=True, stop=True))
                    cx.op("pool", [smb[i2]], [stfb[g]], lambda e: e.tensor_tensor(out=v3(stf[:, g, :]), in0=v3(stf[:, g, :]), in1=sm_[:, 4 + hf, hs].unsqueeze(2).to_broadcast([128, 8, 64]), op=ALU.mult))
                    cx.op("dve", [psb[bk]], [stfb[g]], lambda e: e.tensor_tensor(out=stf[:, g, :], in0=ps[bk][:], in1=stf[:, g, :], op=ALU.add), big=True)
                    cx.op("act", [stfb[g]], [stbb[g]], lambda e: e.copy(out=stb[:, g, :], in_=stf[:, g, :]))
                    yield
                cx.op("pool", [y1sb[k], tyb[k]], [tyb[k]], lambda e: e.tensor_tensor(out=ty[k][:], in0=y1s[k][:], in1=ty[k][:], op=ALU.add))
                cx.op("dve", [tyb[k], szttb[i3]], [yzb[k]], lambda e: e.tensor_tensor(out=yz[k][:], in0=ty[k][:], in1=sztt[i3][:, gcols], op=ALU.mult))
                yield
                cx.op("act", [yzb[k]], [jkb[k], ssqb[k]], lambda e: e.activation(out=jk[k][:], in_=yz[k][:], func=AF.Square, accum_out=ssq[k][:, 0:1]))
                cx.op("act", [ssqb[k]], [ssqb[k]], lambda e: e.activation(out=ssq[k][:, 1:2], in_=ssq[k][:, 0:1], func=AF.Sqrt, bias=self.eps_t[:, 0:1], scale=1.0 / 512.0))
                cx.op("dve", [ssqb[k]], [ssqb[k]], lambda e: e.reciprocal(out=ssq[k][:, 1:2], in_=ssq[k][:, 1:2]))
                cx.op("dve", [yzb[k], ssqb[k]], [ynb[i2]], lambda e: e.tensor_scalar(out=yn[i2][:, gcols], in0=yz[k][:], scalar1=ssq[k][:, 1:2], scalar2=None, op0=ALU.mult))
                yield

            def slot(t, k):
                for g in (k, k + 2):
                    yield from grp(t, g, k)

            def post(t):
                i2 = t % 2
                bi = (t // 4) % 2
                tcols = slice((t % 4) * 128, (t % 4 + 1) * 128)
                for hbk in range(2):
                    pbk = rot[0] % 3
                    rot[0] += 1
                    pv = ps[pbk][:].bitcast(BF16)
                    with cx.group("pe", [ynb[i2], self.cbuf], [psb[pbk]]) as box:
                        for q in range(8):
                            box.append(nc.tensor.transpose(out=pv[:, q * 128:(q + 1) * 128], in_=yn[i2][:, (hbk * 8 + q) * 128:(hbk * 8 + q + 1) * 128], identity=self.ident_b[:]))
                    cx.op("act", [psb[pbk]], [ynTb[bi]], lambda e: e.copy(out=ynT[bi][:, hbk * 8:(hbk + 1) * 8, tcols], in_=pv[:, 0:1024].rearrange("p (q t) -> p q t", t=128)))
                    yield

            def outproj_prefetch(blk):
                bcols = slice(blk * TB, (blk + 1) * TB)
                for fc in range(4):
                    cx.dma("sp", xch[fc][:], self.xs[fc, :, bcols], [self.xs_buf[blk]], [xchb[fc]], xchb[fc])

            def outproj(blk):
                bi = blk % 2
                bcols = slice(blk * TB, (blk + 1) * TB)
                for fc in range(8):
                    pb = rot[0] % 3
                    rot[0] += 1
                    xi = fc % 4
                    if fc >= 4:
                        cx.dma("sp", xch[xi][:], self.xs[fc, :, bcols], [self.xs_buf[blk]], [xchb[xi]], xchb[xi])
                    with cx.group("pe", [wob, ynTb[bi]], [psb[pb]]) as box:
                        for c in range(16):
                            box.append(nc.tensor.matmul(ps[pb][:], lhsT=w_out[:, c, fc * 128:(fc + 1) * 128], rhs=ynT[bi][:, c, :], start=(c == 0), stop=(c == 15)))
                    cx.op("dve", [psb[pb], self.mod_buf[1]], [xchb[xi]], lambda e: e.scalar_tensor_tensor(out=xch[xi][:], in0=ps[pb][:], scalar=self.mod[:, 1, 16 + fc:17 + fc],
                                                                                                         in1=xch[xi][:], op0=ALU.mult, op1=ALU.add))
                    cx.dma("sp", self.xs[fc, :, bcols], xch[xi][:], [xchb[xi]], [self.xs_buf[blk]], self.xs_buf[blk])
                    yield

            load(0)
            if NTL > 1:
                load(1)
            run_threads([pre(0)])
            pending = []
            for t in range(NTL):
                if t + 2 < NTL:
                    load(t + 2)
                th = []
                wts = []
                if t + 1 < NTL:
                    th.append(pre(t + 1))
                    wts.append(8)
                th += [grp(t, g, g) for g in range(4)]
                wts += [1, 1, 1, 1]
                if t > 0:
                    th.append(post(t - 1))
                    wts.append(1)
                th += pending
                wts += [1] * len(pending)
                pending = []
                run_threads(th, wts)
                if t > 0 and (t - 1) % 4 == 3:
                    outproj_prefetch((t - 1) // 4)
                    pending.append(outproj((t - 1) // 4))
            run_threads([post(NTL - 1)] + pending)
            outproj_prefetch((NTL - 1) // 4)
            run_threads([outproj((NTL - 1) // 4)])
            cx.barrier(self.xs_buf + xchb + xctb + szttb + dttb + [wob, pb_, scr])


def _prep_inputs(inputs, b, ntok):
    f = np.float32
    g = lambda k: np.asarray(inputs[k], dtype=f)
    col = lambda v, n: np.ascontiguousarray(v.reshape(n, 128).T)
    m = {}
    m["x"] = np.ascontiguousarray(g("x")[b, :ntok])
    m["c_col"] = col(g("c")[b], 8)
    m["ada_w"] = g("ada_w")
    m["ada_b"] = np.ascontiguousarray(g("ada_b").reshape(2, 1, 6 * D))
    m["norm_mix"] = np.stack([col(g("norm_mix")[i], 8) for i in range(2)])
    m["norm_ffn"] = np.stack([col(g("norm_ffn")[i], 8) for i in range(2)])
    m["norm_final"] = col(g("norm_final"), 8)
    m["gla_w_in"] = g("gla_w_in")[0]
    m["gla_w_gate2"] = g("gla_w_gate2")[0]
    m["gla_b_gate2"] = col(g("gla_b_gate2")[0], 4)
    m["gla_norm"] = col(g("gla_norm")[0], 2)
    m["gla_w_out"] = g("gla_w_out")[0]
    m["ssd_w_in"] = g("ssd_w_in")[0]
    cw = g("ssd_conv_w")[0]
    m["ssd_conv_w"] = np.ascontiguousarray(cw.reshape(4, 24, 128).transpose(2, 1, 0))
    m["ssd_conv_b"] = col(g("ssd_conv_b")[0], 24)
    m["ssd_dt_bias"] = np.ascontiguousarray(g("ssd_dt_bias")[0].reshape(32, 1))
    m["ssd_a_log"] = np.ascontiguousarray(g("ssd_a_log")[0].reshape(32, 1))
    m["ssd_d_bc"] = np.ascontiguousarray(np.broadcast_to(g("ssd_d")[0][None, :], (128, 32)))
    m["ssd_norm_col"] = col(g("ssd_norm")[0], 16)
    m["ssd_w_out"] = g("ssd_w_out")[0]
    m["router_w"] = g("router_w")
    m["router_b_bc"] = np.ascontiguousarray(np.broadcast_to(g("router_b")[None, :], (128, 16)))
    m["moe_w_gate"] = g("moe_w_gate")
    m["moe_w_up"] = g("moe_w_up")
    m["moe_w_down"] = g("moe_w_down")
    return m


def kernel(**inputs):
    ntok = inputs["x"].shape[1]
    nb = inputs["x"].shape[0]
    k = K(ntok)
    nc = k.build()
    in_maps = [_prep_inputs(inputs, b, ntok) for b in range(nb)]
    res = run_bass_kernel_spmd(nc, in_maps, core_ids=list(range(nb)))
    return np.stack([np.asarray(r["out"]) for r in res.results], axis=0).astype(np.float32)
```

```python
import contextlib
import numpy as np
import concourse.bass as bass
import concourse.mybir as mybir
from concourse.bass_utils import run_bass_kernel_spmd

F32 = mybir.dt.float32
BF16 = mybir.dt.bfloat16
AF = mybir.ActivationFunctionType
ALU = mybir.AluOpType
AX = mybir.AxisListType

D = 1024
NCH = 8
TB = 512
EPS = 1e-6
NE = 16
DE = 512
SAME_ENGINE_SYNC = True


class Sem:
    def __init__(self, h, owner=None):
        self.h = h
        self.owner = owner
        self.val = 0


class Buf:
    __slots__ = ("name", "w", "r", "dsem")

    def __init__(self, name):
        self.name = name
        self.w = None
        self.r = {}
        self.dsem = None


class Ctx:
    def __init__(self, nc, es):
        self.nc = nc
        self.es = es
        self.eng = {"pe": nc.tensor, "act": nc.scalar, "dve": nc.vector, "pool": nc.gpsimd, "sp": nc.sync}
        self.sem = {k: Sem(es.enter_context(nc.semaphore("s_" + k)), owner=k) for k in self.eng}
        self.seen = {k: {} for k in self.eng}
        self.nsem = 0

    def _wait(self, e, reads, writes):
        need = {}
        for b in list(reads) + list(writes):
            if b.w is not None:
                s, v, big = b.w
                if s.owner == e and (e == "pe" or big or not SAME_ENGINE_SYNC):
                    continue
                need[s] = max(need.get(s, 0), v)
        for b in writes:
            for s, v in b.r.items():
                if s.owner == e:
                    continue
                need[s] = max(need.get(s, 0), v)
        for s, v in need.items():
            if self.seen[e].get(s, 0) >= v:
                continue
            self.eng[e].wait_ge(s.h, v)
            self.seen[e][s] = v

    def _rec(self, s, v, reads, writes, big=False):
        for b in reads:
            if b.r.get(s, 0) < v:
                b.r[s] = v
        for b in writes:
            b.w = (s, v, big)
            b.r = {}

    def op(self, e, reads, writes, fn, big=False):
        self._wait(e, reads, writes)
        ins = fn(self.eng[e])
        s = self.sem[e]
        s.val += 1
        ins.then_inc(s.h, 1)
        self._rec(s, s.val, reads, writes, big)
        return ins

    @contextlib.contextmanager
    def group(self, e, reads, writes):
        self._wait(e, reads, writes)
        box = []
        yield box
        s = self.sem[e]
        s.val += 1
        box[-1].then_inc(s.h, 1)
        self._rec(s, s.val, reads, writes)

    def dma(self, q, out, in_, reads, writes, on):
        self._wait(q, reads, writes)
        if on.dsem is None:
            self.nsem += 1
            on.dsem = Sem(self.es.enter_context(self.nc.semaphore("d%d" % self.nsem)), owner=None)
        ins = self.eng[q].dma_start(out=out, in_=in_)
        s = on.dsem
        s.val += 16
        ins.then_inc(s.h, 16)
        self._rec(s, s.val, reads, writes)
        return ins

    def barrier(self, bufs=()):
        for e in self.eng:
            for k, s in self.sem.items():
                if k == e or s.val == 0:
                    continue
                if self.seen[e].get(s, 0) >= s.val:
                    continue
                self.eng[e].wait_ge(s.h, s.val)
                self.seen[e][s] = s.val
        for b in bufs:
            need = {}
            if b.w is not None and b.w[0].owner is None:
                need[b.w[0]] = b.w[1]
            for s, v in b.r.items():
                if s.owner is None:
                    need[s] = max(need.get(s, 0), v)
            for e in self.eng:
                for s, v in need.items():
                    if self.seen[e].get(s, 0) < v:
                        self.eng[e].wait_ge(s.h, v)
                        self.seen[e][s] = v


def run_threads(gens, weights=None):
    active = [(g, (weights[i] if weights else 1)) for i, g in enumerate(gens)]
    while active:
        for item in list(active):
            g, w = item
            for _ in range(w):
                try:
                    next(g)
                except StopIteration:
                    active.remove(item)
                    break


class K:
    def __init__(self, ntok, debug=False):
        self.ntok = ntok
        self.nblk = ntok // TB
        self.debug = debug
        self.mask_base = 0
        self.nc = bass.Bass("TRN2", target_bir_lowering=False)
        self.es = contextlib.ExitStack()

    def sb(self, es, name, shape, dt=F32):
        self._uid = getattr(self, "_uid", 0) + 1
        return es.enter_context(self.nc.sbuf_tensor("%s_%d" % (name, self._uid), list(shape), dt))

    def tap(self, name, ap, bufs, dt=F32):
        if not self.debug:
            return
        d = self.nc.dram_tensor("dbg_" + name, list(ap.shape), dt, kind="ExternalOutput").ap()
        b = Buf("dbg_" + name)
        self.cx.dma("sp", d, ap, list(bufs), [b], b)
        self.nc.sync.wait_ge(b.dsem.h, b.dsem.val)

    def din(self, name, shape, dt=F32):
        return self.nc.dram_tensor(name, list(shape), dt, kind="ExternalInput").ap()

    def build(self):
        nc = self.nc
        ntok = self.ntok
        with self.es as es:
            self.cx = cx = Ctx(nc, es)
            I = self.I = {}
            I["x"] = self.din("x", [ntok, D])
            I["c_col"] = self.din("c_col", [128, 8])
            I["ada_w"] = self.din("ada_w", [2, D, 6 * D])
            I["ada_b"] = self.din("ada_b", [2, 1, 6 * D])
            I["norm_mix"] = self.din("norm_mix", [2, 128, 8])
            I["norm_ffn"] = self.din("norm_ffn", [2, 128, 8])
            I["norm_final"] = self.din("norm_final", [128, 8])
            I["gla_w_in"] = self.din("gla_w_in", [D, 3088])
            I["gla_w_gate2"] = self.din("gla_w_gate2", [16, 512])
            I["gla_b_gate2"] = self.din("gla_b_gate2", [128, 4])
            I["gla_norm"] = self.din("gla_norm", [128, 2])
            I["gla_w_out"] = self.din("gla_w_out", [D, D])
            I["ssd_w_in"] = self.din("ssd_w_in", [D, 5152])
            I["ssd_conv_w"] = self.din("ssd_conv_w", [128, 24, 4])
            I["ssd_conv_b"] = self.din("ssd_conv_b", [128, 24])
            I["ssd_dt_bias"] = self.din("ssd_dt_bias", [32, 1])
            I["ssd_a_log"] = self.din("ssd_a_log", [32, 1])
            I["ssd_d_bc"] = self.din("ssd_d_bc", [128, 32])
            I["ssd_norm_col"] = self.din("ssd_norm_col", [128, 16])
            I["ssd_w_out"] = self.din("ssd_w_out", [2048, D])
            I["router_w"] = self.din("router_w", [D, 16])
            I["router_b_bc"] = self.din("router_b_bc", [128, 16])
            I["moe_w_gate"] = self.din("moe_w_gate", [2, NE, D, DE])
            I["moe_w_up"] = self.din("moe_w_up", [2, NE, D, DE])
            I["moe_w_down"] = self.din("moe_w_down", [2, NE, DE, D])
            self.out = nc.dram_tensor("out", [ntok, D], F32, kind="ExternalOutput").ap()
            kind = "ExternalOutput" if self.debug else "Internal"
            self.xs = nc.dram_tensor("xs", [NCH, 128, ntok], F32, kind=kind).ap()
            self.modrow = nc.dram_tensor("modrow", [2, 6 * D], F32, kind=kind).ap()
            self.xs_buf = [Buf("xs%d" % b) for b in range(self.nblk)]
            self.out_buf = Buf("out")

            self.ps = [es.enter_context(nc.psum_tensor("ps%d" % i, [128, 512], F32)) for i in range(8)]
            self.psb = [Buf("psb%d" % i) for i in range(8)]

            self.consts(es)
            self.phase_mod(0)
            self.phase_mod(1)
            self.phase_xin()
            cx.barrier(self.xs_buf)
            self.phases()
            s = self.out_buf.dsem
            if s is not None:
                nc.sync.wait_ge(s.h, s.val)
        return nc

    def consts(self, es):
        nc, cx = self.nc, self.cx
        self.ident_f = self.sb(es, "ident_f", [128, 128], F32)
        self.ident_b = self.sb(es, "ident_b", [128, 128], BF16)
        self.ones_b = self.sb(es, "ones_b", [128, 128], BF16)
        self.cbuf = Buf("consts")
        g = nc.gpsimd
        g.memset(self.ident_f[:], 1.0)
        g.affine_select(out=self.ident_f[:], in_=self.ident_f[:], pattern=[[-1, 128]],
                        compare_op=ALU.is_equal, fill=0.0, base=0, channel_multiplier=1)
        g.tensor_copy(out=self.ident_b[:], in_=self.ident_f[:])
        self.eps_t = self.sb(es, "eps_t", [128, 1], F32)
        g.memset(self.eps_t[:], EPS)
        cx.op("pool", [], [self.cbuf], lambda e: e.memset(self.ones_b[:], 1.0))
        self.mod = self.sb(es, "mod", [128, 2, 48], F32)
        self.mod_buf = [Buf("mod0"), Buf("mod1")]
        self.cond = self.sb(es, "cond", [128, 8], F32)
        self.cond_buf = Buf("cond")
        c_raw = self.sb(es, "c_raw", [128, 8], F32)
        cb = Buf("c_raw")
        cx.dma("sp", c_raw[:], self.I["c_col"], [], [cb], cb)
        cx.op("act", [cb], [self.cond_buf], lambda e: e.activation(out=self.cond[:], in_=c_raw[:], func=AF.Silu))
        self.gains = self.sb(es, "gains", [128, 5, 8], F32)
        self.gains_buf = Buf("gains")
        for i in range(2):
            cx.dma("sp", self.gains[:, i, :], self.I["norm_mix"][i], [], [self.gains_buf], self.gains_buf)
            cx.dma("sp", self.gains[:, 2 + i, :], self.I["norm_ffn"][i], [], [self.gains_buf], self.gains_buf)
        cx.dma("sp", self.gains[:, 4, :], self.I["norm_final"], [], [self.gains_buf], self.gains_buf)
        self.AB = self.sb(es, "AB", [128, 2, 2, 2, 8], F32)
        self.AB_buf = [[Buf("AB%d%d" % (i, j)) for j in range(2)] for i in range(2)]

    def phase_mod(self, layer):
        nc, cx = self.nc, self.cx
        with contextlib.ExitStack() as es:
            wbufs = [self.sb(es, "adaw%d" % i, [128, 8, 512], F32) for i in range(2)]
            wb = [Buf("adaw%d" % i) for i in range(2)]
            brow = self.sb(es, "adab", [1, 6 * D], F32)
            bb = Buf("adab")
            row = self.sb(es, "modrow_sb", [1, 6 * D], F32)
            rb = Buf("modrow_sb")
            cx.dma("sp", brow[:], self.I["ada_b"][layer], [], [bb], bb)
            for j in range(12):
                w = wbufs[j % 2]
                cx.dma("sp", w[:], self.I["ada_w"][layer, :, j * 512:(j + 1) * 512].rearrange("(c p) n -> p c n", p=128),
                       [], [wb[j % 2]], wb[j % 2])
                pb = j % 2
                with cx.group("pe", [wb[j % 2], self.cond_buf], [self.psb[pb]]) as box:
                    for c in range(8):
                        box.append(nc.tensor.matmul(self.ps[pb][0:1, :], lhsT=self.cond[:, c:c + 1], rhs=w[:, c, :],
                                                    start=(c == 0), stop=(c == 7)))
                cx.op("dve", [self.psb[pb], bb], [rb],
                      lambda e: e.tensor_tensor(out=row[0:1, j * 512:(j + 1) * 512], in0=self.ps[pb][0:1, :],
                                                in1=brow[0:1, j * 512:(j + 1) * 512], op=ALU.add))
            mrb = Buf("modrow_dram")
            cx.dma("sp", self.modrow[layer:layer + 1, :], row[:], [rb], [mrb], mrb)
            with nc.allow_non_contiguous_dma(reason="tiny mod vector relayout"):
                cx.dma("sp", self.mod[:, layer, :], self.modrow[layer].rearrange("(j p) -> p j", p=128),
                       [mrb], [self.mod_buf[layer]], self.mod_buf[layer])
            for which in range(2):
                gi = (0 if which == 0 else 2) + layer
                sh = self.mod[:, layer, which * 24: which * 24 + 8]
                sc = self.mod[:, layer, which * 24 + 8: which * 24 + 16]
                A = self.AB[:, layer, which, 0, :]
                B = self.AB[:, layer, which, 1, :]
                ab = self.AB_buf[layer][which]
                cx.op("dve", [self.mod_buf[layer], self.gains_buf], [ab],
                      lambda e: e.scalar_tensor_tensor(out=A, in0=sc, scalar=1.0, in1=self.gains[:, gi, :],
                                                       op0=ALU.add, op1=ALU.mult))
                cx.op("dve", [self.mod_buf[layer]], [ab], lambda e: e.tensor_copy(out=B, in_=sh))
            cx.barrier([bb, rb, mrb] + wb)

    def phase_xin(self):
        nc, cx = self.nc, self.cx
        with contextlib.ExitStack() as es:
            xt = [self.sb(es, "xin%d" % i, [128, D], F32) for i in range(2)]
            xtb = [Buf("xin%d" % i) for i in range(2)]
            xT = [self.sb(es, "xT%d" % i, [128, NCH, TB], F32) for i in range(2)]
            xTb = [Buf("xT%d" % i) for i in range(2)]
            for blk in range(self.nblk):
                o = xT[blk % 2]
                ob = xTb[blk % 2]
                for tt in range(4):
                    n = blk * 4 + tt
                    t = xt[n % 2]
                    tb = xtb[n % 2]
                    cx.dma("sp", t[:], self.I["x"][n * 128:(n + 1) * 128, :], [], [tb], tb)
                    for half in range(2):
                        pb = (n * 2 + half) % 4
                        with cx.group("pe", [tb, self.cbuf], [self.psb[pb]]) as box:
                            for q in range(4):
                                c = half * 4 + q
                                box.append(nc.tensor.transpose(out=self.ps[pb][:, q * 128:(q + 1) * 128],
                                                               in_=t[:, c * 128:(c + 1) * 128], identity=self.ident_f[:]))
                        src = self.ps[pb][:].rearrange("p (q t) -> p q t", t=128)
                        dst = o[:, half * 4:(half + 1) * 4, tt * 128:(tt + 1) * 128]
                        if half == 0:
                            cx.op("act", [self.psb[pb]], [ob], lambda e: e.copy(out=dst, in_=src))
                        else:
                            cx.op("dve", [self.psb[pb]], [ob], lambda e: e.tensor_copy(out=dst, in_=src))
                cx.dma("sp", self.xs[:, :, blk * TB:(blk + 1) * TB].rearrange("c p t -> p c t"), o[:],
                       [ob], [self.xs_buf[blk]], self.xs_buf[blk])
            cx.barrier(xtb + xTb)

    def phases(self):
        plan = self.plan if getattr(self, "plan", None) else ["gla", "moe0", "ssd", "moe1", "final"]
        for p in plan:
            if p == "gla":
                self.phase_gla()
            elif p == "ssd":
                self.phase_ssd()
            elif p == "moe0":
                self.phase_moe(0)
            elif p == "moe1":
                self.phase_moe(1)
            elif p == "final":
                self.phase_final()
            self.cx.barrier(self.xs_buf)

    def norm_block(self, *a, **kw):
        for _ in self.norm_block_gen(*a, **kw):
            pass

    def norm_block_gen(self, W, x, xb, A, B, ab, h, hb, sq, sqb, tmp, tmpb, s_t, sb_, pbank, gain_only=False):
        nc, cx = self.nc, self.cx
        cx.op("act", [xb], [sqb], lambda e: e.activation(out=sq, in_=x, func=AF.Square))
        yield
        with cx.group("pe", [sqb, self.cbuf], [self.psb[pbank]]) as box:
            for c in range(8):
                box.append(nc.tensor.matmul(self.ps[pbank][:, 0:W], lhsT=self.ones_b[:], rhs=sq[:, c, :],
                                            start=(c == 0), stop=(c == 7)))
        cx.op("act", [self.psb[pbank]], [sb_],
              lambda e: e.activation(out=s_t[:, 0, 0:W], in_=self.ps[pbank][:, 0:W], func=AF.Sqrt, bias=self.eps_t[:, 0:1], scale=1.0 / D))
        yield
        cx.op("dve", [sb_], [sb_], lambda e: e.reciprocal(out=s_t[:, 1, 0:W], in_=s_t[:, 0, 0:W]), big=True)
        for c in range(8):
            cx.op("dve", [xb, sb_, ab], [tmpb],
                  lambda e: e.scalar_tensor_tensor(out=tmp[:, c, :], in0=x[:, c, :], scalar=A[:, c:c + 1],
                                                   in1=s_t[:, 1, 0:W], op0=ALU.mult, op1=ALU.mult))
        yield
        if gain_only:
            return
        for c in range(8):
            cx.op("act", [tmpb, ab], [hb],
                  lambda e: e.activation(out=h[:, c, :], in_=tmp[:, c, :], func=AF.Identity, bias=B[:, c:c + 1], scale=1.0))

    def phase_moe(self, layer):
        nc, cx = self.nc, self.cx
        HT = min(2048, self.ntok)
        nhalf = self.ntok // HT
        nb = HT // TB
        NT = HT // 128
        W = 256
        A = self.AB[:, layer, 1, 0, :]
        B = self.AB[:, layer, 1, 1, :]
        ab = self.AB_buf[layer][1]
        with contextlib.ExitStack() as es:
            xacc = self.sb(es, "xacc", [128, NCH, HT], F32)
            xab = [Buf("xacc%d" % i) for i in range(nb)]
            h2 = self.sb(es, "h2", [128, NCH, HT], BF16)
            h2b = [Buf("h2_%d" % i) for i in range(nb)]
            wg = [self.sb(es, "wg%d" % i, [128, 8, DE], BF16) for i in range(2)]
            wu = [self.sb(es, "wu%d" % i, [128, 8, DE], BF16) for i in range(2)]
            wd = [self.sb(es, "wd%d" % i, [128, 4, D], BF16) for i in range(2)]
            wgb = [Buf("wg%d" % i) for i in range(2)]
            wub = [Buf("wu%d" % i) for i in range(2)]
            wdb = [Buf("wd%d" % i) for i in range(2)]
            tmp2 = [self.sb(es, "ntmp%d" % i_, [128, NCH, W], F32) for i_ in range(2)]
            tmpb2 = [Buf("ntmp%d" % i_) for i_ in range(2)]
            sq2_ = [self.sb(es, "nsq%d" % i_, [128, NCH, W], BF16) for i_ in range(2)]
            sqb2 = [Buf("nsq%d" % i_) for i_ in range(2)]
            s_t2 = [self.sb(es, "ns%d" % i_, [128, 2, W], F32) for i_ in range(2)]
            sb2_ = [Buf("ns%d" % i_) for i_ in range(2)]
            he = [self.sb(es, "he%d" % i, [128, 4, TB], BF16) for i in range(2)]
            heb = [Buf("he%d" % i) for i in range(2)]
            sg = self.sb(es, "sg", [128, TB], F32)
            sgb = Buf("sg")
            t1 = self.sb(es, "t1", [128, TB], F32)
            t1b = Buf("t1")
            Gs = self.sb(es, "Gs", [128, TB], F32)
            Gsb = Buf("Gs")
            rw = self.sb(es, "rw", [128, NCH, 16], F32)
            rwb = Buf("rw")
            rb = self.sb(es, "rb", [128, 16], F32)
            b16 = self.sb(es, "b16", [16, 1], F32)
            b16b = Buf("b16")
            selt = [self.sb(es, "sel%d" % i_, [16, 128], F32) for i_ in range(2)]
            seltb = [Buf("sel%d" % i_) for i_ in range(2)]
            gT = self.sb(es, "gT", [16, HT], F32)
            gTb = Buf("gT")
            lgT = gT
            lgTb = gTb
            R = [self.sb(es, "rt%d" % i, [128, NT, 16], F32) for i in range(6)]
            Rb = [Buf("rt%d" % i) for i in range(6)]
            Rs = [self.sb(es, "rs%d" % i, [128, NT * 4], F32) for i in range(4)]
            Rsb = [Buf("rs%d" % i) for i in range(4)]

            cx.dma("sp", rw[:], self.I["router_w"].rearrange("(c p) e -> p c e", p=128), [], [rwb], rwb)
            cx.dma("sp", rb[:], self.I["router_b_bc"], [], [rwb], rwb)
            with cx.group("pe", [rwb, ab], [self.psb[6]]) as box:
                for c in range(8):
                    box.append(nc.tensor.matmul(self.ps[6][0:16, 0:1], lhsT=rw[:, c, :], rhs=B[:, c:c + 1], start=(c == 0), stop=(c == 7)))
            cx.op("act", [self.psb[6]], [b16b], lambda e: e.copy(out=b16[:], in_=self.ps[6][0:16, 0:1]))
            ones16 = self.sb(es, "ones16", [16, 128], F32)
            o16b = Buf("ones16")
            cx.op("pool", [], [o16b], lambda e: e.memset(ones16[:], 1.0))

            def load_w(e, half):
                i = (half * NE + e) % 2
                cx.dma("pool", wg[i][:], self.I["moe_w_gate"][layer, e].rearrange("(c p) n -> p c n", p=128), [], [wgb[i]], wgb[i])
                cx.dma("pool", wu[i][:], self.I["moe_w_up"][layer, e].rearrange("(c p) n -> p c n", p=128), [], [wub[i]], wub[i])
                cx.dma("pool", wd[i][:], self.I["moe_w_down"][layer, e].rearrange("(c p) n -> p c n", p=128), [], [wdb[i]], wdb[i])

            for half in range(nhalf):
                t0 = half * HT
                load_w(0, half)
                for blk in range(nb):
                    gb = half * nb + blk
                    cx.dma("sp", xacc[:, :, blk * TB:(blk + 1) * TB],
                           self.xs[:, :, t0 + blk * TB: t0 + (blk + 1) * TB].rearrange("c p t -> p c t"),
                           [self.xs_buf[gb]], [xab[blk]], xab[blk])
                subs = [(blk, sub) for blk in range(nb) for sub in range(TB // W)]
                pend = None
                for n_ in range(len(subs) + 1):
                    cur = None
                    if n_ < len(subs):
                        blk, sub = subs[n_]
                        c0 = blk * TB + sub * W
                        si = n_ % 2
                        tmp, tmpb, sq, sqb, s_t, sb_ = tmp2[si], tmpb2[si], sq2_[si], sqb2[si], s_t2[si], sb2_[si]
                        g_ = self.norm_block_gen(W, xacc[:, :, c0:c0 + W], xab[blk], A, B, ab, h2[:, :, c0:c0 + W], h2b[blk],
                                                 sq[:], sqb, tmp[:], tmpb, s_t, sb_, 4 if si == 0 else 7)
                        for _ in range(3):
                            next(g_)
                        cur = (g_, tmp, tmpb, c0, 6 if si == 0 else 3)
                    if pend is not None:
                        g_, tmp, tmpb, c0, prb = pend
                        for _ in g_:
                            pass
                        with cx.group("pe", [tmpb, rwb], [self.psb[prb]]) as box:
                            for c in range(8):
                                box.append(nc.tensor.matmul(self.ps[prb][0:16, 0:W], lhsT=rw[:, c, :], rhs=tmp[:, c, :], start=(c == 0), stop=(c == 7)))
                        cx.op("act", [self.psb[prb], b16b], [lgTb], lambda e: e.activation(out=lgT[:, c0:c0 + W], in_=self.ps[prb][0:16, 0:W], func=AF.Identity, bias=b16[:, 0:1], scale=1.0))
                    pend = cur
                with cx.group("pe", [lgTb, self.cbuf], [self.psb[5]]) as box:
                    for tt in range(NT):
                        box.append(nc.tensor.transpose(out=self.ps[5][:, tt * 16:(tt + 1) * 16], in_=lgT[0:16, tt * 128:(tt + 1) * 128], identity=self.ident_f[0:16, 0:16]))
                NE4 = NT * 4
                sgm, bi, mb, eq, mb2, gates = R
                m1, m2, gs, gsel = Rs
                v3 = lambda t: t[:].rearrange("p t (g k) -> p (t g) k", k=4)
                f2 = lambda t: t[:].rearrange("p t e -> p (t e)")
                cx.op("act", [self.psb[5]], [Rb[0]], lambda e: e.activation(out=f2(sgm), in_=self.ps[5][:, 0:NT * 16], func=AF.Sigmoid))
                cx.op("dve", [Rb[0], rwb], [Rb[1]], lambda e: e.tensor_tensor(out=bi[:], in0=sgm[:], in1=rb[:].unsqueeze(1).to_broadcast([128, NT, 16]), op=ALU.add))
                cx.op("dve", [Rb[1]], [Rsb[0]], lambda e: e.tensor_reduce(out=m1[:], in_=v3(bi), axis=AX.X, op=ALU.max))
                cx.op("dve", [Rb[1], Rsb[0]], [Rb[3]], lambda e: e.tensor_tensor(out=v3(eq), in0=v3(bi), in1=m1[:].unsqueeze(2).to_broadcast([128, NE4, 4]), op=ALU.is_equal))
                cx.op("dve", [Rb[3], Rb[1]], [Rb[4]], lambda e: e.scalar_tensor_tensor(out=mb2[:], in0=eq[:], scalar=-10.0, in1=bi[:], op0=ALU.mult, op1=ALU.add))
                cx.op("dve", [Rb[4]], [Rsb[1]], lambda e: e.tensor_reduce(out=m2[:], in_=v3(mb2), axis=AX.X, op=ALU.max))
                cx.op("dve", [Rsb[0], Rsb[1]], [Rsb[2]], lambda e: e.tensor_tensor(out=gs[:], in0=m1[:], in1=m2[:], op=ALU.add))
                gs3 = gs[:].rearrange("p (t g) -> p t g", g=4)
                cx.op("dve", [Rsb[2]], [Rsb[1]], lambda e: e.tensor_reduce(out=m2[:, 0:NT], in_=gs3, axis=AX.X, op=ALU.max))
                cx.op("dve", [Rsb[2], Rsb[1]], [Rsb[3]], lambda e: e.tensor_tensor(out=gsel[:].rearrange("p (t g) -> p t g", g=4), in0=gs3,
                                                                                    in1=m2[:, 0:NT].unsqueeze(2).to_broadcast([128, NT, 4]), op=ALU.is_equal))
                cx.op("dve", [Rb[1], Rsb[3]], [Rb[2]], lambda e: e.scalar_tensor_tensor(out=v3(mb), in0=v3(bi), scalar=2.0,
                                                                                        in1=gsel[:].unsqueeze(2).to_broadcast([128, NE4, 4]), op0=ALU.add, op1=ALU.mult))
                cx.op("dve", [Rb[2]], [Rsb[0]], lambda e: e.tensor_reduce(out=m1[:, 0:NT], in_=mb[:], axis=AX.X, op=ALU.max))
                cx.op("dve", [Rb[2], Rsb[0]], [Rb[3]], lambda e: e.tensor_tensor(out=eq[:], in0=mb[:], in1=m1[:, 0:NT].unsqueeze(2).to_broadcast([128, NT, 16]), op=ALU.is_equal))
                cx.op("dve", [Rb[3], Rb[2]], [Rb[4]], lambda e: e.scalar_tensor_tensor(out=mb2[:], in0=eq[:], scalar=-10.0, in1=mb[:], op0=ALU.mult, op1=ALU.add))
                cx.op("dve", [Rb[4]], [Rsb[1]], lambda e: e.tensor_reduce(out=m2[:, 0:NT], in_=mb2[:], axis=AX.X, op=ALU.max))
                cx.op("dve", [Rb[4], Rsb[1]], [Rb[2]], lambda e: e.tensor_tensor(out=mb[:], in0=mb2[:], in1=m2[:, 0:NT].unsqueeze(2).to_broadcast([128, NT, 16]), op=ALU.is_equal))
                cx.op("dve", [Rb[2], Rb[3]], [Rb[3]], lambda e: e.tensor_tensor(out=eq[:], in0=eq[:], in1=mb[:], op=ALU.add))
                cx.op("dve", [Rb[3], Rb[0]], [Rb[2]], lambda e: e.tensor_tensor(out=mb[:], in0=eq[:], in1=sgm[:], op=ALU.mult))
                cx.op("dve", [Rb[2]], [Rsb[0]], lambda e: e.tensor_reduce(out=m1[:, 0:NT], in_=mb[:], axis=AX.X, op=ALU.add))
                cx.op("dve", [Rsb[0]], [Rsb[1]], lambda e: e.reciprocal(out=m2[:, 0:NT], in_=m1[:, 0:NT]))
                cx.op("dve", [Rb[2], Rsb[1]], [Rb[5]], lambda e: e.tensor_tensor(out=gates[:], in0=mb[:], in1=m2[:, 0:NT].unsqueeze(2).to_broadcast([128, NT, 16]), op=ALU.mult))
                if half == 0:
                    self.tap("sgm", sgm[:], [Rb[0]])
                    self.tap("gates", gates[:], [Rb[5]])
                    self.tap("bi", bi[:], [Rb[1]])
                    self.tap("gs", gs[:], [Rsb[2]])
                    self.tap("gsel", gsel[:], [Rsb[3]])
                for g4 in range(NT // 4):
                    with cx.group("pe", [Rb[5], self.cbuf], [self.psb[6]]) as box:
                        for q in range(4):
                            box.append(nc.tensor.transpose(out=self.ps[6][0:16, q * 128:(q + 1) * 128], in_=gates[:, g4 * 4 + q, :], identity=self.ident_f[:]))
                    cx.op("act", [self.psb[6]], [gTb], lambda e: e.copy(out=gT[:, g4 * 512:(g4 + 1) * 512], in_=self.ps[6][0:16, :]))
                it = 0
                dn_pending = [None]
                for ex in range(NE):
                    if ex + 1 < NE:
                        if dn_pending[0] is not None:
                            dn_pending[0]()
                            dn_pending[0] = None
                        load_w(ex + 1, half)
                    wi = (half * NE + ex) % 2
                    sel_e = selt[ex % 2]
                    selb = seltb[ex % 2]
                    cx.op("pool", [o16b], [selb], lambda e: e.affine_select(out=sel_e[:], in_=ones16[:], pattern=[[0, 128]], compare_op=ALU.is_equal,
                                                                          fill=0.0, base=-ex, channel_multiplier=1))
                    for blk in range(nb):
                        cols = slice(blk * TB, (blk + 1) * TB)
                        hb_i = it % 2
                        cx.op("pe", [selb, gTb], [self.psb[4]],
                              lambda e: e.matmul(self.ps[4][:], lhsT=sel_e[:], rhs=gT[:, cols], start=True, stop=True))
                        cx.op("act", [self.psb[4]], [Gsb], lambda e: e.copy(out=Gs[:], in_=self.ps[4][:]))
                        for dc in range(4):
                            pg = dc % 2
                            pu = 2 + dc % 2
                            with cx.group("pe", [wgb[wi], h2b[blk]], [self.psb[pg]]) as box:
                                for c in range(8):
                                    box.append(nc.tensor.matmul(self.ps[pg][:], lhsT=wg[wi][:, c, dc * 128:(dc + 1) * 128], rhs=h2[:, c, cols],
                                                                start=(c == 0), stop=(c == 7)))
                            with cx.group("pe", [wub[wi], h2b[blk]], [self.psb[pu]]) as box:
                                for c in range(8):
                                    box.append(nc.tensor.matmul(self.ps[pu][:], lhsT=wu[wi][:, c, dc * 128:(dc + 1) * 128], rhs=h2[:, c, cols],
                                                                start=(c == 0), stop=(c == 7)))
                            cx.op("act", [self.psb[pg]], [sgb], lambda e: e.activation(out=sg[:], in_=self.ps[pg][:], func=AF.Silu))
                            cx.op("dve", [sgb, Gsb], [t1b], lambda e: e.tensor_tensor(out=t1[:], in0=sg[:], in1=Gs[:], op=ALU.mult), big=True)
                            cx.op("dve", [t1b, self.psb[pu]], [heb[hb_i]], lambda e: e.tensor_tensor(out=he[hb_i][:, dc, :], in0=self.ps[pu][:], in1=t1[:], op=ALU.mult))
                        def down(wi=wi, hb_i=hb_i, blk=blk, cols=cols):
                            for fc in range(8):
                                pd = 5 + fc % 3
                                with cx.group("pe", [wdb[wi], heb[hb_i]], [self.psb[pd]]) as box:
                                    for dc in range(4):
                                        box.append(nc.tensor.matmul(self.ps[pd][:], lhsT=wd[wi][:, dc, fc * 128:(fc + 1) * 128], rhs=he[hb_i][:, dc, :],
                                                                    start=(dc == 0), stop=(dc == 3)))
                                cx.op("dve", [self.psb[pd], self.mod_buf[layer]], [xab[blk]],
                                      lambda e: e.scalar_tensor_tensor(out=xacc[:, fc, cols], in0=self.ps[pd][:], scalar=self.mod[:, layer, 40 + fc:41 + fc],
                                                                       in1=xacc[:, fc, cols], op0=ALU.mult, op1=ALU.add))
                        if dn_pending[0] is not None:
                            dn_pending[0]()
                        dn_pending[0] = down
                        it += 1
                if dn_pending[0] is not None:
                    dn_pending[0]()
                    dn_pending[0] = None
                for blk in range(nb):
                    gb = half * nb + blk
                    cx.dma("sp", self.xs[:, :, t0 + blk * TB: t0 + (blk + 1) * TB].rearrange("c p t -> p c t"),
                           xacc[:, :, blk * TB:(blk + 1) * TB], [xab[blk]], [self.xs_buf[gb]], self.xs_buf[gb])
            cx.barrier(self.xs_buf + xab + wgb + wub + wdb + [rwb])

    def phase_final(self):
        nc, cx = self.nc, self.cx
        with contextlib.ExitStack() as es:
            NB_ = 3
            xb_t = [self.sb(es, "fx%d" % i, [128, NCH, TB], F32) for i in range(NB_)]
            xbb = [Buf("fx%d" % i) for i in range(NB_)]
            tmp = [self.sb(es, "ftmp%d" % i, [128, NCH, TB], F32) for i in range(2)]
            tmpb = [Buf("ftmp%d" % i) for i in range(2)]
            sq = [self.sb(es, "fsq%d" % i, [128, NCH, TB], BF16) for i in range(2)]
            sqb = [Buf("fsq%d" % i) for i in range(2)]
            s_t = [self.sb(es, "fs%d" % i, [128, 2, TB], F32) for i in range(2)]
            sb_ = [Buf("fs%d" % i) for i in range(2)]
            ot = [self.sb(es, "fo%d" % i, [128, D], F32) for i in range(3)]
            otb = [Buf("fo%d" % i) for i in range(3)]

            def load(n):
                cx.dma("sp", xb_t[n % NB_][:], self.xs[:, :, n * TB:(n + 1) * TB].rearrange("c p t -> p c t"), [self.xs_buf[n]], [xbb[n % NB_]], xbb[n % NB_])

            def back(blk):
                i2 = blk % 2
                for tt in range(4):
                    n = blk * 4 + tt
                    o = ot[n % 3]
                    ob = otb[n % 3]
                    for half in range(2):
                        pb = (n * 2 + half) % 4
                        with cx.group("pe", [tmpb[i2], self.cbuf], [self.psb[pb]]) as box:
                            for q in range(4):
                                c = half * 4 + q
                                box.append(nc.tensor.transpose(out=self.ps[pb][:, q * 128:(q + 1) * 128],
                                                               in_=tmp[i2][:, c, tt * 128:(tt + 1) * 128], identity=self.ident_f[:]))
                        if half == 0:
                            cx.op("act", [self.psb[pb]], [ob], lambda e: e.copy(out=o[:, 0:512], in_=self.ps[pb][:]))
                        else:
                            cx.op("dve", [self.psb[pb]], [ob], lambda e: e.tensor_copy(out=o[:, 512:1024], in_=self.ps[pb][:]))
                    cx.dma("sp", self.out[n * 128:(n + 1) * 128, :], o[:], [ob], [self.out_buf], ob)

            for n in range(min(2, self.nblk)):
                load(n)
            for n in range(self.nblk + 1):
                if n < self.nblk:
                    if n + 2 < self.nblk:
                        load(n + 2)
                    i2 = n % 2
                    self.norm_block(TB, xb_t[n % NB_][:], xbb[n % NB_], self.gains[:, 4, :], None, self.gains_buf, None, None, sq[i2][:], sqb[i2],
                                    tmp[i2][:], tmpb[i2], s_t[i2], sb_[i2], 4 + i2, gain_only=True)
                if n > 0:
                    back(n - 1)
            cx.barrier(xbb + otb + [self.out_buf])
            for b_ in otb:
                if b_.dsem is not None:
                    nc.sync.wait_ge(b_.dsem.h, b_.dsem.val)

    def phase_gla(self):
        nc, cx = self.nc, self.cx
        A = self.AB[:, 0, 0, 0, :]
        B = self.AB[:, 0, 0, 1, :]
        ab = self.AB_buf[0][0]
        I = self.I
        with contextlib.ExitStack() as es:
            w_in = self.sb(es, "gwin", [128, 8, 3088], BF16)
            w_out = self.sb(es, "gwout", [128, 8, D], BF16)
            wb = Buf("gw")
            wg2 = self.sb(es, "gwg2", [16, 512], F32)
            bg2 = self.sb(es, "gbg2", [128, 4], F32)
            nbg2 = self.sb(es, "gnbg2", [128, 4], F32)
            gn = self.sb(es, "ggn", [128, 2], F32)
            lns = self.sb(es, "glns", [128, 1], F32)
            mask = self.sb(es, "gmask", [128, 64], F32)
            cmask = self.sb(es, "gcmask", [128, TB], F32)
            pb_ = Buf("gparams")
            for c in range(8):
                cx.dma("pool", w_in[:, c, :], I["gla_w_in"][c * 128:(c + 1) * 128, :], [], [wb], wb)
            cx.dma("pool", w_out[:], I["gla_w_out"].rearrange("(c p) n -> p c n", p=128), [], [wb], wb)
            cx.dma("sp", wg2[:], I["gla_w_gate2"], [], [pb_], pb_)
            cx.dma("sp", bg2[:], I["gla_b_gate2"], [], [pb_], pb_)
            cx.dma("sp", gn[:], I["gla_norm"], [], [pb_], pb_)
            cx.op("dve", [pb_], [pb_], lambda e: e.tensor_scalar(out=nbg2[:], in0=bg2[:], scalar1=-1.0, scalar2=None, op0=ALU.mult))
            cx.op("pool", [], [pb_], lambda e: e.memset(lns[:], float(np.log(128.0 ** -0.5))))
            cx.op("pool", [], [pb_], lambda e: e.memset(mask[:], 1.0))
            cx.op("pool", [pb_], [pb_], lambda e: e.affine_select(out=mask[0:64, :], in_=mask[0:64, :], pattern=[[1, 64]], compare_op=ALU.is_ge,
                                                                  fill=0.0, base=0, channel_multiplier=-1))
            cx.op("pool", [pb_], [pb_], lambda e: e.affine_select(out=mask[64:128, :], in_=mask[64:128, :], pattern=[[1, 64]], compare_op=ALU.is_ge,
                                                                  fill=0.0, base=self.mask_base, channel_multiplier=-1))
            cx.op("pool", [pb_], [pb_], lambda e: e.memset(cmask[:], 1.0))
            cx.op("pool", [pb_], [pb_], lambda e: e.memset(cmask[:].rearrange("p (c t) -> p c t", t=64)[:, :, 0:1], 0.0))
            self.tap("gmask", mask[:], [pb_])

            NW = 128
            xblk = self.sb(es, "gx", [128, NCH, TB], F32); xbb = Buf("gx")
            h = self.sb(es, "gh", [128, NCH, TB], BF16); hb = Buf("gh")
            tmpc = [self.sb(es, "gtmpc%d" % i_, [128, TB], F32) for i_ in range(2)]; tmpcb = [Buf("gtmpc%d" % i_) for i_ in range(2)]
            rs_ = self.sb(es, "grs", [128, TB], F32); sb_ = Buf("grs")
            glr = self.sb(es, "gglr", [16, TB], F32); glrb = Buf("gglr")
            e1 = self.sb(es, "ge1", [128, TB], F32); e1b = Buf("ge1")
            nla = self.sb(es, "gnla", [128, TB], F32); nlab = Buf("gnla")
            Bc = self.sb(es, "gBc", [128, TB], F32); Bcb = Buf("gBc")
            Eq = self.sb(es, "gEq", [128, TB], F32); Eqb = Buf("gEq")
            Ek = self.sb(es, "gEk", [128, TB], F32); Ekb = Buf("gEk")
            D2 = lambda nm, shp, dt: ([self.sb(es, nm + str(i_), shp, dt) for i_ in range(2)], [Buf(nm + str(i_)) for i_ in range(2)])
            dec_, decb_ = D2("gdec", [128, 4, 8], F32)
            qk_, qkb_ = D2("gqk", [128, 8, TB], BF16)
            v_, vb_ = D2("gv", [128, 4, D], BF16)
            sr_, srb_ = D2("gsr", [128, 8, TB], BF16)
            ktok_, ktb_ = D2("gktok", [128, 4, 512], BF16)
            Sm_, Smb_ = D2("gSm", [128, 16, 64], BF16)
            stf = self.sb(es, "gstf", [128, 4, 256], F32); stfb = [Buf("gstf%d" % i_) for i_ in range(2)]
            stb = self.sb(es, "gstb", [128, 4, 256], BF16); stbb = [Buf("gstb%d" % i_) for i_ in range(2)]
            sq2 = [self.sb(es, "gsq2%d" % i_, [128, 4, 256], BF16) for i_ in range(2)]; sq2b = [Buf("gsq2%d" % i_) for i_ in range(2)]
            s2 = [self.sb(es, "gs2%d" % i_, [128, TB], F32) for i_ in range(2)]; s2b = [Buf("gs2%d" % i_) for i_ in range(2)]
            t2 = [self.sb(es, "gt2%d" % i_, [128, 256], F32) for i_ in range(2)]; t2b = [Buf("gt2%d" % i_) for i_ in range(2)]
            og = self.sb(es, "gog", [128, 8, TB], BF16); ogb = Buf("gog")
            oraw = [self.sb(es, "goraw%d" % i_, [128, 4, 256], F32) for i_ in range(2)]; orawb = [Buf("goraw%d" % i_) for i_ in range(2)]
            for k2 in range(2):
                cx.op("pool", [], [stfb[k2]], lambda e: e.memset(stf[:, 2 * k2:2 * k2 + 2, :], 0.0))
                cx.op("pool", [], [stbb[k2]], lambda e: e.memset(stb[:, 2 * k2:2 * k2 + 2, :], 0.0))
            ps, psb = self.ps, self.psb
            rot = [0]

            def inproj(lhs_fn, rhs_fn, M=128):
                pb = rot[0] % 2
                rot[0] += 1
                with cx.group("pe", [wb, hb], [psb[pb]]) as box:
                    for c in range(8):
                        box.append(nc.tensor.matmul(ps[pb][0:M, :], lhsT=lhs_fn(c), rhs=rhs_fn(c), start=(c == 0), stop=(c == 7)))
                return pb

            def stageA(blk):
                si = blk % 2
                dec, decb, qk, qkb, v, vb = dec_[si], decb_[si], qk_[si], qkb_[si], v_[si], vb_[si]
                sr, srb, ktok, ktb, Sm, Smb = sr_[si], srb_[si], ktok_[si], ktb_[si], Sm_[si], Smb_[si]
                bcols = slice(blk * TB, (blk + 1) * TB)
                cx.dma("sp", xblk[:], self.xs[:, :, bcols].rearrange("c p t -> p c t"), [self.xs_buf[blk]], [xbb], xbb)
                for _ in range(5):
                    yield
                cx.op("act", [xbb], [hb], lambda e: e.activation(out=h[:], in_=xblk[:], func=AF.Square))
                yield
                yield
                pbn = rot[0] % 2
                rot[0] += 1
                with cx.group("pe", [hb, self.cbuf], [psb[pbn]]) as box:
                    for c in range(8):
                        box.append(nc.tensor.matmul(ps[pbn][:], lhsT=self.ones_b[:], rhs=h[:, c, :], start=(c == 0), stop=(c == 7)))
                cx.op("act", [psb[pbn]], [sb_], lambda e: e.activation(out=rs_[:], in_=ps[pbn][:], func=AF.Sqrt, bias=self.eps_t[:, 0:1], scale=1.0 / D))
                yield
                cx.op("dve", [sb_], [sb_], lambda e: e.reciprocal(out=rs_[:], in_=rs_[:]), big=True)
                yield
                for c in range(8):
                    ti = c % 2
                    cx.op("dve", [xbb, sb_, ab], [tmpcb[ti]], lambda e: e.scalar_tensor_tensor(out=tmpc[ti][:], in0=xblk[:, c, :], scalar=A[:, c:c + 1], in1=rs_[:], op0=ALU.mult, op1=ALU.mult))
                    cx.op("act", [tmpcb[ti], ab], [hb], lambda e: e.activation(out=h[:, c, :], in_=tmpc[ti][:], func=AF.Identity, bias=B[:, c:c + 1], scale=1.0))
                    if c % 2 == 1:
                        yield
                pb = inproj(lambda c: w_in[:, c, 3072:3088], lambda c: h[:, c, :], M=16)
                cx.op("act", [psb[pb]], [glrb], lambda e: e.copy(out=glr[:], in_=ps[pb][0:16, :]))
                yield
                for j in range(4):
                    pb = rot[0] % 2
                    rot[0] += 1
                    cx.op("pe", [pb_, glrb], [psb[pb]], lambda e: e.matmul(ps[pb][:], lhsT=wg2[:, j * 128:(j + 1) * 128], rhs=glr[:], start=True, stop=True))
                    cx.op("act", [psb[pb], pb_], [e1b], lambda e: e.activation(out=e1[:], in_=ps[pb][:], func=AF.Exp, bias=nbg2[:, j:j + 1], scale=-1.0))
                    cx.op("act", [e1b], [nlab], lambda e: e.activation(out=nla[:], in_=e1[:], func=AF.Ln, bias=1.0, scale=1.0))
                    cx.op("dve", [nlab, pb_], [Bcb], lambda e: e.tensor_tensor_scan(out=Bc[:], data0=cmask[:], data1=nla[:], initial=0.0, op0=ALU.mult, op1=ALU.add))
                    cx.op("act", [Bcb, pb_], [Eqb], lambda e: e.activation(out=Eq[:], in_=Bc[:], func=AF.Exp, bias=lns[:, 0:1], scale=-1.0 / 16.0))
                    cx.op("act", [Bcb], [Ekb], lambda e: e.activation(out=Ek[:], in_=Bc[:], func=AF.Exp, scale=1.0 / 16.0))
                    cx.op("act", [Bcb], [decb], lambda e: e.activation(out=dec[:, j, :], in_=Bc[:].rearrange("p (c t) -> p c t", t=64)[:, :, 63], func=AF.Exp, scale=-1.0 / 16.0))
                    yield
                    pb = inproj(lambda c: w_in[:, c, j * 128:(j + 1) * 128], lambda c: h[:, c, :])
                    cx.op("dve", [psb[pb], Eqb], [qkb], lambda e: e.tensor_tensor(out=qk[:, j, :], in0=ps[pb][:], in1=Eq[:], op=ALU.mult))
                    pb = inproj(lambda c: w_in[:, c, (4 + j) * 128:(5 + j) * 128], lambda c: h[:, c, :])
                    cx.op("dve", [psb[pb], Ekb], [qkb], lambda e: e.tensor_tensor(out=qk[:, 4 + j, :], in0=ps[pb][:], in1=Ek[:], op=ALU.mult))
                    yield
                for tt in range(4):
                    for hf in range(2):
                        pb = inproj(lambda c: h[:, c, tt * 128:(tt + 1) * 128], lambda c: w_in[:, c, 1024 + hf * 512: 1024 + (hf + 1) * 512])
                        cx.op("act", [psb[pb]], [vb], lambda e: e.copy(out=v[:, tt, hf * 512:(hf + 1) * 512], in_=ps[pb][:]))
                        yield
                for j in range(8):
                    pb = inproj(lambda c: w_in[:, c, 2048 + j * 128: 2048 + (j + 1) * 128], lambda c: h[:, c, :])
                    cx.op("act", [psb[pb]], [srb], lambda e: e.activation(out=sr[:, j, :], in_=ps[pb][:], func=AF.Silu))
                    yield
                for tt in range(4):
                    pbk = rot[0] % 2
                    rot[0] += 1
                    pvb = ps[pbk][:].bitcast(BF16)
                    with cx.group("pe", [qkb, self.cbuf], [psb[pbk]]) as box:
                        for hd in range(4):
                            box.append(nc.tensor.transpose(out=pvb[:, hd * 128:(hd + 1) * 128], in_=qk[:, 4 + hd, tt * 128:(tt + 1) * 128], identity=self.ident_b[:]))
                    cx.op("act", [psb[pbk]], [ktb], lambda e: e.copy(out=ktok[:, tt, :], in_=pvb[:, 0:512]))
                    yield
                for bank in range(2):
                    pbk = rot[0] % 2
                    rot[0] += 1
                    with cx.group("pe", [qkb], [psb[pbk]]) as box:
                        for t2_ in range(2):
                            tt = bank * 2 + t2_
                            for hd in range(4):
                                for hf in range(2):
                                    cc = slice(tt * 128 + hf * 64, tt * 128 + hf * 64 + 64)
                                    o0 = (t2_ * 4 + hd) * 64
                                    box.append(nc.tensor.matmul(ps[pbk][hf * 64:(hf + 1) * 64, o0:o0 + 64], lhsT=qk[:, 4 + hd, cc], rhs=qk[:, hd, cc], start=True, stop=True))
                    cx.op("dve", [psb[pbk], pb_], [Smb],
                          lambda e: e.tensor_tensor(out=Sm[:, bank * 8:(bank + 1) * 8, :], in0=ps[pbk][:].rearrange("p (a t) -> p a t", t=64),
                                                    in1=mask[:].unsqueeze(1).to_broadcast([128, 8, 64]), op=ALU.mult))
                    yield

            def pair_thread(blk, k):
                si = blk % 2
                dec, decb, qk, qkb, v, vb = dec_[si], decb_[si], qk_[si], qkb_[si], v_[si], vb_[si]
                sr, srb, ktok, ktb, Sm, Smb = sr_[si], srb_[si], ktok_[si], ktb_[si], Sm_[si], Smb_[si]
                bO = [2 + 3 * k, 3 + 3 * k]
                bP = 4 + 3 * k
                pO = [ps[b_][:].rearrange("p (j t) -> p j t", t=256) for b_ in bO]
                pP = ps[bP][:].rearrange("p (h v) -> p h v", v=256)
                stf_p = stf[:, 2 * k:2 * k + 2, :]
                stb_p = stb[:, 2 * k:2 * k + 2, :]
                post_steps = []
                for q in range(2):
                    for c4 in range(4):
                        c = q * 4 + c4
                        tt, hf = c // 2, c % 2
                        rows = slice(hf * 64, (hf + 1) * 64)
                        cc = slice(c * 64, (c + 1) * 64)
                        wc = slice(c4 * 64, (c4 + 1) * 64)
                        with cx.group("pe", [stbb[k], qkb, vb, Smb, ktb], [psb[bO[0]], psb[bO[1]], psb[bP]]) as box:
                            for hd2 in range(2):
                                hd = 2 * k + hd2
                                for j in range(2):
                                    box.append(nc.tensor.matmul(pO[hd2][:, j, wc], lhsT=stb[:, hd, j * 128:(j + 1) * 128], rhs=qk[:, hd, cc], start=True, stop=False))
                                    box.append(nc.tensor.matmul(pO[hd2][:, j, wc], lhsT=v[rows, tt, hd * 256 + j * 128: hd * 256 + (j + 1) * 128],
                                                                rhs=Sm[rows, tt * 4 + hd, :], start=False, stop=True))
                                box.append(nc.tensor.matmul(pP[:, hd2, :], lhsT=ktok[rows, tt, hd * 128:(hd + 1) * 128],
                                                            rhs=v[rows, tt, hd * 256:(hd + 1) * 256], start=True, stop=True))
                        yield
                        cx.op("dve", [psb[bP]], [stfb[k]], lambda e: e.tensor_tensor(out=stf_p, in0=pP, in1=stf_p, op=ALU.add), big=True)
                        cx.op("dve", [decb], [stfb[k]], lambda e: e.tensor_tensor(out=stf_p, in0=stf_p, in1=dec[:, 2 * k:2 * k + 2, c:c + 1].to_broadcast([128, 2, 256]), op=ALU.mult), big=True)
                        yield
                        cx.op("act", [stfb[k]], [stbb[k]], lambda e: e.copy(out=stb_p, in_=stf_p))
                        if post_steps:
                            post_steps.pop(0)()
                        yield
                    while post_steps:
                        post_steps.pop(0)()
                    qc = slice(q * 256, (q + 1) * 256)
                    for hd2 in range(2):
                        cx.op("act", [psb[bO[hd2]]], [sq2b[k]], lambda e: e.activation(out=sq2[k][:, hd2 * 2:hd2 * 2 + 2, :], in_=pO[hd2], func=AF.Square))
                        cx.op("act", [psb[bO[hd2]]], [orawb[k]], lambda e: e.copy(out=oraw[k][:, hd2 * 2:hd2 * 2 + 2, :], in_=pO[hd2]))
                    yield

                    def p1(qc=qc):
                        pbs = rot[0] % 2
                        rot[0] += 1
                        with cx.group("pe", [sq2b[k], self.cbuf], [psb[pbs]]) as box:
                            for hd2 in range(2):
                                for j in range(2):
                                    box.append(nc.tensor.matmul(ps[pbs][:, hd2 * 256:(hd2 + 1) * 256], lhsT=self.ones_b[:], rhs=sq2[k][:, hd2 * 2 + j, :], start=(j == 0), stop=(j == 1)))
                        cx.op("act", [psb[pbs]], [s2b[k]], lambda e: e.activation(out=s2[k][:], in_=ps[pbs][:], func=AF.Sqrt, bias=self.eps_t[:, 0:1], scale=1.0 / 256.0))
                        cx.op("dve", [s2b[k]], [s2b[k]], lambda e: e.reciprocal(out=s2[k][:], in_=s2[k][:]), big=True)

                    def p2(hd2, qc=qc):
                        hd = 2 * k + hd2
                        for j in range(2):
                            cx.op("dve", [orawb[k], s2b[k], pb_], [t2b[k]], lambda e: e.scalar_tensor_tensor(out=t2[k][:], in0=oraw[k][:, hd2 * 2 + j, :], scalar=gn[:, j:j + 1],
                                                                                                            in1=s2[k][:, hd2 * 256:(hd2 + 1) * 256], op0=ALU.mult, op1=ALU.mult), big=True)
                            cx.op("dve", [t2b[k], srb], [ogb], lambda e: e.tensor_tensor(out=og[:, hd * 2 + j, qc], in0=t2[k][:], in1=sr[:, hd * 2 + j, qc], op=ALU.mult))

                    post_steps.extend([p1, lambda: p2(0), lambda: p2(1)])
                while post_steps:
                    post_steps.pop(0)()
                    yield

            def stageC(blk):
                bcols = slice(blk * TB, (blk + 1) * TB)
                cx.dma("sp", xblk[:], self.xs[:, :, bcols].rearrange("c p t -> p c t"), [self.xs_buf[blk]], [xbb], xbb)
                for fc in range(8):
                    pb = rot[0] % 2
                    rot[0] += 1
                    with cx.group("pe", [wb, ogb], [psb[pb]]) as box:
                        for c in range(8):
                            box.append(nc.tensor.matmul(ps[pb][:], lhsT=w_out[:, c, fc * 128:(fc + 1) * 128], rhs=og[:, c, :], start=(c == 0), stop=(c == 7)))
                    cx.op("dve", [psb[pb], self.mod_buf[0]], [xbb], lambda e: e.scalar_tensor_tensor(out=xblk[:, fc, :], in0=ps[pb][:], scalar=self.mod[:, 0, 16 + fc:17 + fc],
                                                                                                    in1=xblk[:, fc, :], op0=ALU.mult, op1=ALU.add))
                cx.dma("sp", self.xs[:, :, bcols].rearrange("c p t -> p c t"), xblk[:], [xbb], [self.xs_buf[blk]], self.xs_buf[blk])

            run_threads([stageA(0)])
            for blk in range(self.nblk):
                if blk > 0:
                    stageC(blk - 1)
                th = [pair_thread(blk, 0), pair_thread(blk, 1)]
                wts = [1, 1]
                if blk + 1 < self.nblk:
                    th.append(stageA(blk + 1))
                    wts.append(3)
                run_threads(th, wts)
            stageC(self.nblk - 1)
            cx.barrier(self.xs_buf + [xbb, wb, pb_])

    def phase_ssd(self):
        nc = self.nc
        NTL = self.ntok // 128
        self.xcs = nc.dram_tensor("xcs", [NTL, 128, 24, 128], BF16).ap()
        self.zs = nc.dram_tensor("zs", [NTL, 128, 2048], BF16).ap()
        self.dts = nc.dram_tensor("dts", [NTL, 32, 2, 128], F32).ap()
        self.scr = Buf("ssd_scr")
        self.phase_ssd_a()
        self.phase_ssd_b()

    def phase_ssd_a(self):
        nc, cx = self.nc, self.cx
        A = self.AB[:, 1, 0, 0, :]
        B = self.AB[:, 1, 0, 1, :]
        ab = self.AB_buf[1][0]
        I = self.I
        W = 256
        ntok = self.ntok
        scr = self.scr
        with contextlib.ExitStack() as es:
            hall = self.sb(es, "sha", [128, NCH, ntok], BF16); hab = Buf("sha")
            xsub = [self.sb(es, "sxs%d" % i, [128, NCH, W], F32) for i in range(4)]; xsubb = [Buf("sxs%d" % i) for i in range(4)]
            tmp = [self.sb(es, "stmp%d" % i, [128, NCH, W], F32) for i in range(4)]; tmpb = [Buf("stmp%d" % i) for i in range(4)]
            sq = [self.sb(es, "ssq%d" % i, [128, NCH, W], BF16) for i in range(4)]; sqb = [Buf("ssq%d" % i) for i in range(4)]
            s_t = [self.sb(es, "ss_t%d" % i, [128, 2, W], F32) for i in range(4)]; sb_ = [Buf("ss_t%d" % i) for i in range(4)]
            wgrp = [self.sb(es, "swg%d" % i, [128, 8, 512], BF16) for i in range(3)]
            wgb = [Buf("swg%d" % i) for i in range(3)]
            cw = self.sb(es, "scw", [128, 24, 4], F32)
            cbias = self.sb(es, "scb", [128, 24], F32)
            dtb = self.sb(es, "sdtb", [32, 1], F32)
            alog = self.sb(es, "salog", [32, 1], F32)
            aneg = self.sb(es, "saneg", [32, 1], F32)
            hist = self.sb(es, "shist", [128, 3], BF16); histb = Buf("shist")
            pb_ = Buf("sparams")
            cx.dma("sp", cw[:], I["ssd_conv_w"], [], [pb_], pb_)
            cx.dma("sp", cbias[:], I["ssd_conv_b"], [], [pb_], pb_)
            cx.dma("sp", dtb[:], I["ssd_dt_bias"], [], [pb_], pb_)
            cx.dma("sp", alog[:], I["ssd_a_log"], [], [pb_], pb_)
            cx.op("act", [pb_], [pb_], lambda e: e.activation(out=aneg[:], in_=alog[:], func=AF.Exp))
            cx.op("dve", [pb_], [pb_], lambda e: e.tensor_scalar(out=aneg[:], in0=aneg[:], scalar1=-1.0, scalar2=None, op0=ALU.mult))
            u = [self.sb(es, "su%d" % i, [128, 515], BF16) for i in range(2)]; ub = [Buf("su%d" % i) for i in range(2)]
            acc = [self.sb(es, "sacc%d" % i, [128, TB], F32) for i in range(2)]; accb = [Buf("sacc%d" % i) for i in range(2)]
            xcq = [self.sb(es, "sxcq%d" % i, [128, TB], BF16) for i in range(3)]; xcqb = [Buf("sxcq%d" % i) for i in range(3)]
            zt = [self.sb(es, "szt%d" % i, [128, TB], BF16) for i in range(3)]; ztb = [Buf("szt%d" % i) for i in range(3)]
            e1 = self.sb(es, "se1", [32, TB], F32); e1b = Buf("se1")
            dtT = [self.sb(es, "sdtT%d" % i, [32, 2, TB], F32) for i in range(2)]; dtTb = [Buf("sdtT%d" % i) for i in range(2)]
            ps, psb = self.ps, self.psb
            rot = [0]
            wrot = [0]

            def load_grp(c0, ncols):
                i = wrot[0] % 3
                wrot[0] += 1
                cx.dma("pool", wgrp[i][:, :, 0:ncols], I["ssd_w_in"][:, c0:c0 + ncols].rearrange("(c p) n -> p c n", p=128), [], [wgb[i]], wgb[i])
                return i

            nxt = load_grp(0, 512)
            n = 0
            gens_ = []
            nsub = self.nblk * (TB // W)

            def issue_load(n):
                blk, sub = divmod(n, TB // W)
                c0 = blk * TB + sub * W
                i2 = n % 4
                cx.dma("sp", xsub[i2][:], self.xs[:, :, c0:c0 + W].rearrange("c p t -> p c t"), [self.xs_buf[blk]], [xsubb[i2]], xsubb[i2])

            for n in range(min(3, nsub)):
                issue_load(n)
            pend = None
            for n in range(nsub + 1):
                cur = None
                if n < nsub:
                    blk, sub = divmod(n, TB // W)
                    c0 = blk * TB + sub * W
                    i2 = n % 4
                    if n + 3 < nsub:
                        issue_load(n + 3)
                    g_ = self.norm_block_gen(W, xsub[i2][:], xsubb[i2], A, B, ab, hall[:, :, c0:c0 + W], hab, sq[i2][:], sqb[i2], tmp[i2][:], tmpb[i2], s_t[i2], sb_[i2], 2 + i2)
                    for _ in range(3):
                        next(g_)
                    cur = g_
                if pend is not None:
                    for _ in pend:
                        pass
                pend = cur
            nz = 0
            for zg in range(4):
                wi = nxt
                nxt = load_grp((zg + 1) * 512, 512)
                for blk in range(self.nblk):
                    for tt in range(4):
                        tile_ = blk * 4 + tt
                        pb = rot[0] % 2
                        rot[0] += 1
                        with cx.group("pe", [wgb[wi], hab], [psb[pb]]) as box:
                            for c in range(8):
                                box.append(nc.tensor.matmul(ps[pb][:], lhsT=hall[:, c, tile_ * 128:(tile_ + 1) * 128], rhs=wgrp[wi][:, c, :], start=(c == 0), stop=(c == 7)))
                        zi = nz % 3
                        nz += 1
                        cx.op("act", [psb[pb]], [ztb[zi]], lambda e: e.activation(out=zt[zi][:], in_=ps[pb][:], func=AF.Silu))
                        cx.dma("sp", self.zs[tile_, :, zg * 512:(zg + 1) * 512], zt[zi][:], [ztb[zi]], [scr], ztb[zi])
            nq = 0
            deferred = [None]
            for xg in range(6):
                wi = nxt
                nxt = load_grp(2048 + (xg + 1) * 512, 512) if xg < 5 else load_grp(5120, 32)
                for q in range(4):
                    ch = xg * 4 + q
                    cx.op("dve", [], [histb], lambda e: e.memset(hist[:], 0.0))
                    for blk in range(self.nblk):
                        bcols = slice(blk * TB, (blk + 1) * TB)
                        pb = rot[0] % 2
                        rot[0] += 1
                        ui = nq % 2
                        xi = nq % 3
                        nq += 1
                        with cx.group("pe", [wgb[wi], hab], [psb[pb]]) as box:
                            for c in range(8):
                                box.append(nc.tensor.matmul(ps[pb][:], lhsT=wgrp[wi][:, c, q * 128:(q + 1) * 128], rhs=hall[:, c, bcols], start=(c == 0), stop=(c == 7)))
                        cx.op("act", [histb], [ub[ui]], lambda e: e.copy(out=u[ui][:, 0:3], in_=hist[:]))
                        cx.op("act", [psb[pb]], [ub[ui]], lambda e: e.copy(out=u[ui][:, 3:515], in_=ps[pb][:]))
                        cx.op("act", [ub[ui]], [histb], lambda e: e.copy(out=hist[:], in_=u[ui][:, 512:515]))
                        cx.op("act", [psb[pb], pb_], [accb[ui]], lambda e: e.activation(out=acc[ui][:], in_=ps[pb][:], func=AF.Identity, scale=cw[:, ch, 3:4]))
                        for j in range(3):
                            cx.op("dve", [ub[ui], pb_], [accb[ui]], lambda e: e.scalar_tensor_tensor(out=acc[ui][:], in0=u[ui][:, j:j + 512], scalar=cw[:, ch, j:j + 1],
                                                                                                     in1=acc[ui][:], op0=ALU.mult, op1=ALU.add), big=True)
                        def fin(ui=ui, xi=xi, ch=ch, blk=blk):
                            cx.op("act", [accb[ui], pb_], [xcqb[xi]], lambda e: e.activation(out=xcq[xi][:], in_=acc[ui][:], func=AF.Silu, bias=cbias[:, ch:ch + 1], scale=1.0))
                            cx.dma("sp", self.xcs[blk * 4:(blk + 1) * 4, :, ch, :].rearrange("t p k -> p t k"), xcq[xi][:].rearrange("p (t k) -> p t k", k=128),
                                   [xcqb[xi]], [scr], xcqb[xi])
                        if deferred[0] is not None:
                            deferred[0]()
                        deferred[0] = fin
            if deferred[0] is not None:
                deferred[0]()
            wi = nxt
            for blk in range(self.nblk):
                bcols = slice(blk * TB, (blk + 1) * TB)
                pb = rot[0] % 2
                rot[0] += 1
                di = blk % 2
                with cx.group("pe", [wgb[wi], hab], [psb[pb]]) as box:
                    for c in range(8):
                        box.append(nc.tensor.matmul(ps[pb][0:32, :], lhsT=wgrp[wi][:, c, 0:32], rhs=hall[:, c, bcols], start=(c == 0), stop=(c == 7)))
                cx.op("act", [psb[pb], pb_], [e1b], lambda e: e.activation(out=e1[:], in_=ps[pb][0:32, :], func=AF.Exp, bias=dtb[:, 0:1], scale=1.0))
                cx.op("act", [e1b], [dtTb[di]], lambda e: e.activation(out=dtT[di][:, 0, :], in_=e1[:], func=AF.Ln, bias=1.0, scale=1.0))
                cx.op("dve", [dtTb[di], pb_], [dtTb[di]], lambda e: e.tensor_scalar(out=dtT[di][:, 1, :], in0=dtT[di][:, 0, :], scalar1=aneg[:, 0:1], scalar2=None, op0=ALU.mult))
                for a_ in range(2):
                    cx.dma("sp", self.dts[blk * 4:(blk + 1) * 4, :, a_, :].rearrange("t h k -> h t k"), dtT[di][:, a_, :].rearrange("h (t k) -> h t k", k=128),
                           [dtTb[di]], [scr], dtTb[di])
            cx.barrier([scr, pb_] + ztb + xcqb + dtTb + wgb + xsubb + self.xs_buf)

    def phase_ssd_b(self):
        nc, cx = self.nc, self.cx
        I = self.I
        NTL = self.ntok // 128
        scr = self.scr
        with contextlib.ExitStack() as es:
            w_out = self.sb(es, "swout", [128, 16, D], BF16)
            wob = Buf("swout")
            cx.dma("pool", w_out[:], I["ssd_w_out"].rearrange("(c p) n -> p c n", p=128), [], [wob], wob)
            dbc = self.sb(es, "sdbc", [128, 32], F32)
            ngcol = self.sb(es, "sngcol", [128, 16], F32)
            mask = self.sb(es, "smask", [128, 64], F32)
            imask = self.sb(es, "simask", [128, 64], F32)
            ones2 = self.sb(es, "sones2", [128, 128], F32)
            onesh = self.sb(es, "sonesh", [128, 2, 128], F32)
            tri2 = self.sb(es, "stri2", [128, 128], F32)
            Dsk = self.sb(es, "sDsk", [128, 32, 64], BF16)
            pb_ = Buf("sparams")
            cx.dma("sp", dbc[:], I["ssd_d_bc"], [], [pb_], pb_)
            cx.dma("sp", ngcol[:], I["ssd_norm_col"], [], [pb_], pb_)
            for c in range(16):
                cx.op("dve", [pb_, wob], [wob], lambda e: e.tensor_scalar(out=w_out[:, c, :], in0=w_out[:, c, :], scalar1=ngcol[:, c:c + 1], scalar2=None, op0=ALU.mult))
            P_ = lambda fn: cx.op("pool", [pb_], [pb_], fn)
            P_(lambda e: e.memset(mask[:], 1.0))
            for hf in range(2):
                r_ = slice(hf * 64, (hf + 1) * 64)
                P_(lambda e: e.affine_select(out=mask[r_, :], in_=mask[r_, :], pattern=[[1, 64]], compare_op=ALU.is_ge, fill=0.0, base=0, channel_multiplier=-1))
            P_(lambda e: e.memset(imask[:], 1.0))
            for hf in range(2):
                r_ = slice(hf * 64, (hf + 1) * 64)
                P_(lambda e: e.affine_select(out=imask[r_, :], in_=imask[r_, :], pattern=[[1, 64]], compare_op=ALU.is_equal, fill=0.0, base=0, channel_multiplier=-1))
            P_(lambda e: e.memset(ones2[:], 0.0))
            P_(lambda e: e.memset(onesh[:], 0.0))
            P_(lambda e: e.memset(tri2[:], 0.0))
            for hf in range(2):
                r_ = slice(hf * 64, (hf + 1) * 64)
                P_(lambda e: e.memset(ones2[r_, hf * 64:(hf + 1) * 64], 1.0))
                P_(lambda e: e.memset(onesh[r_, hf, :], 1.0))
                P_(lambda e: e.tensor_copy(out=tri2[r_, hf * 64:(hf + 1) * 64], in_=mask[r_, :]))
            cx.op("dve", [pb_], [pb_], lambda e: e.tensor_tensor(out=Dsk[:], in0=dbc[:].unsqueeze(2).to_broadcast([128, 32, 64]),
                                                                 in1=imask[:].unsqueeze(1).to_broadcast([128, 32, 64]), op=ALU.mult))
            L3 = lambda nm, shp, dt: ([self.sb(es, nm + str(i), shp, dt) for i in range(3)], [Buf(nm + str(i)) for i in range(3)])
            L2 = lambda nm, shp, dt: ([self.sb(es, nm + str(i), shp, dt) for i in range(2)], [Buf(nm + str(i)) for i in range(2)])
            L4 = lambda nm, shp, dt: ([self.sb(es, nm + str(i), shp, dt) for i in range(4)], [Buf(nm + str(i)) for i in range(4)])
            xct, xctb = L3("sxct", [128, 24, 128], BF16)
            sztt, szttb = L3("sszt", [128, 2048], BF16)
            dtt, dttb = L3("sdtt", [32, 2, 128], F32)
            xch, xchb = L4("sxch", [128, TB], F32)
            xtok, xtokb = L2("sxtok", [128, 2048], BF16)
            Btok, Btokb = L2("sBtok", [128, 512], BF16)
            dd, ddb = L2("sdd", [128, 2, 32], F32)
            sm, smb = L2("ssm", [128, 8, 32], F32)
            CBm, CBmb = L2("sCBm", [128, 4, 64], F32)
            Dm, Dmb = L2("sDm", [128, 8, 64], F32)
            dif, difb = L2("sdif", [128, 8, 64], F32)
            Mt4, _ = L2("sMt", [128, 32, 64], BF16)
            xdt4, _ = L2("sxdt", [128, 2048], BF16)
            xw4, _ = L2("sxw", [128, 2048], BF16)
            Mt4b = [[Buf("sMt%d_%d" % (i_, g_)) for g_ in range(4)] for i_ in range(2)]
            xdt4b = [[Buf("sxdt%d_%d" % (i_, g_)) for g_ in range(4)] for i_ in range(2)]
            xw4b = [[Buf("sxw%d_%d" % (i_, g_)) for g_ in range(4)] for i_ in range(2)]
            ty, tyb = L4("sty", [128, 512], F32)
            yz, yzb = L4("syz", [128, 512], F32)
            y1s, y1sb = L4("sy1s", [128, 512], F32)
            jk, jkb = L2("sjk", [128, 512], BF16)
            jk = jk + jk; jkb = jkb + jkb
            ssq, ssqb = L4("sssq", [128, 2], F32)
            yn, ynb = L2("syn", [128, 2048], BF16)
            ynT, ynTb = L2("synT", [128, 16, TB], BF16)
            stf = self.sb(es, "sstf", [128, 4, 512], F32); stfb = [Buf("sstf%d" % i) for i in range(4)]
            stb = self.sb(es, "sstb", [128, 4, 512], BF16); stbb = [Buf("sstb%d" % i) for i in range(4)]
            for g in range(4):
                cx.op("pool", [], [stfb[g]], lambda e: e.memset(stf[:, g, :], 0.0))
                cx.op("pool", [], [stbb[g]], lambda e: e.memset(stb[:, g, :], 0.0))
            ps, psb = self.ps, self.psb
            rot = [0]

            def load(t):
                i3 = t % 3
                cx.dma("sp", xct[i3][:], self.xcs[t], [scr], [xctb[i3]], xctb[i3])
                cx.dma("sp", sztt[i3][:], self.zs[t], [scr], [szttb[i3]], szttb[i3])
                cx.dma("sp", dtt[i3][:], self.dts[t], [scr], [dttb[i3]], dttb[i3])

            def pre(t):
                i2 = t % 2
                i3 = t % 3
                xc_ = xct[i3]
                xcb = xctb[i3]
                for hbk in range(2):
                    pbk = rot[0] % 3
                    rot[0] += 1
                    pv = ps[pbk][:].bitcast(BF16)
                    with cx.group("pe", [xcb, self.cbuf], [psb[pbk]]) as box:
                        for q in range(8):
                            box.append(nc.tensor.transpose(out=pv[:, q * 128:(q + 1) * 128], in_=xc_[:, hbk * 8 + q, :], identity=self.ident_b[:]))
                    cx.op("act", [psb[pbk]], [xtokb[i2]], lambda e: e.copy(out=xtok[i2][:, hbk * 1024:(hbk + 1) * 1024], in_=pv[:, 0:1024]))
                    yield
                pbk = rot[0] % 3
                rot[0] += 1
                pv5 = ps[pbk][:].bitcast(BF16)
                with cx.group("pe", [xcb, self.cbuf], [psb[pbk]]) as box:
                    for g in range(4):
                        box.append(nc.tensor.transpose(out=pv5[:, g * 128:(g + 1) * 128], in_=xc_[:, 16 + g, :], identity=self.ident_b[:]))
                cx.op("act", [psb[pbk]], [Btokb[i2]], lambda e: e.copy(out=Btok[i2][:], in_=pv5[:, 0:512]))
                yield
                p4 = ps[3]
                b4 = psb[3]
                with cx.group("pe", [dttb[i3], self.cbuf], [b4]) as box:
                    for k2 in range(2):
                        box.append(nc.tensor.transpose(out=p4[:, k2 * 32:(k2 + 1) * 32], in_=dtt[i3][:, k2, :], identity=self.ident_f[0:32, 0:32]))
                yield
                cx.op("act", [b4], [ddb[i2]], lambda e: e.copy(out=dd[i2][:].rearrange("p a h -> p (a h)"), in_=p4[:, 0:64]))
                yield
                with cx.group("pe", [ddb[i2], pb_, xcb], [b4]) as box:
                    box.append(nc.tensor.matmul(p4[:, 0:32], lhsT=tri2[:], rhs=dd[i2][:, 1, :], start=True, stop=True))
                    box.append(nc.tensor.matmul(p4[:, 32:64], lhsT=ones2[:], rhs=dd[i2][:, 1, :], start=True, stop=True))
                    for hf in range(2):
                        box.append(nc.tensor.matmul(p4[:, 64 + hf * 32: 96 + hf * 32], lhsT=onesh[:, hf, :], rhs=dd[i2][:, 1, :], start=True, stop=True))
                    for g in range(4):
                        for hf in range(2):
                            cc = slice(hf * 64, hf * 64 + 64)
                            box.append(nc.tensor.matmul(p4[hf * 64:(hf + 1) * 64, 128 + g * 64: 192 + g * 64], lhsT=xc_[:, 16 + g, cc], rhs=xc_[:, 20 + g, cc], start=True, stop=True))
                yield
                sm_ = sm[i2]
                sb2 = smb[i2]
                cx.op("act", [b4], [sb2], lambda e: e.copy(out=sm_[:, 0, :], in_=p4[:, 0:32]))
                cx.op("act", [b4], [sb2], lambda e: e.activation(out=sm_[:, 1, :], in_=p4[:, 0:32], func=AF.Exp))
                cx.op("act", [b4], [sb2], lambda e: e.activation(out=sm_[:, 4:6, :].rearrange("p a h -> p (a h)"), in_=p4[:, 64:128], func=AF.Exp))
                cx.op("dve", [b4, sb2], [sb2], lambda e: e.tensor_tensor(out=sm_[:, 6, :], in0=p4[:, 32:64], in1=sm_[:, 0, :], op=ALU.subtract))
                cx.op("act", [sb2], [sb2], lambda e: e.activation(out=sm_[:, 2, :], in_=sm_[:, 6, :], func=AF.Exp))
                cx.op("dve", [sb2, ddb[i2]], [sb2], lambda e: e.tensor_tensor(out=sm_[:, 3, :], in0=sm_[:, 2, :], in1=dd[i2][:, 0, :], op=ALU.mult))
                cx.op("dve", [b4, pb_], [CBmb[i2]], lambda e: e.tensor_tensor(out=CBm[i2][:], in0=p4[:, 128:384].rearrange("p (g t) -> p g t", t=64),
                                                                             in1=mask[:].unsqueeze(1).to_broadcast([128, 4, 64]), op=ALU.mult))
                yield
                v3 = lambda ap: ap.rearrange("p (h q) -> p h q", q=64)
                for g in range(4):
                    k = g % 2
                    hs = slice(g * 8, (g + 1) * 8)
                    gcols = slice(g * 512, (g + 1) * 512)
                    cx.op("pool", [ddb[i2], pb_], [Dmb[k]], lambda e: e.tensor_tensor(out=Dm[k][:], in0=dd[i2][:, 1, hs].unsqueeze(2).to_broadcast([128, 8, 64]),
                                                                                     in1=mask[:].unsqueeze(1).to_broadcast([128, 8, 64]), op=ALU.mult))
                    cx.op("pool", [xtokb[i2], ddb[i2]], [xdt4b[i2][g]], lambda e: e.tensor_tensor(out=v3(xdt4[i2][:, gcols]), in0=v3(xtok[i2][:, gcols]), in1=dd[i2][:, 0, hs].unsqueeze(2).to_broadcast([128, 8, 64]), op=ALU.mult))
                    cx.op("pool", [xtokb[i2], sb2], [xw4b[i2][g]], lambda e: e.tensor_tensor(out=v3(xw4[i2][:, gcols]), in0=v3(xtok[i2][:, gcols]), in1=sm_[:, 3, hs].unsqueeze(2).to_broadcast([128, 8, 64]), op=ALU.mult))
                    yield
                    pbk = rot[0] % 3
                    rot[0] += 1
                    cx.op("pe", [Dmb[k], pb_], [psb[pbk]], lambda e: e.matmul(ps[pbk][:], lhsT=ones2[:], rhs=Dm[k][:].rearrange("p h t -> p (h t)"), start=True, stop=True))
                    cx.op("dve", [psb[pbk], sb2], [difb[k]], lambda e: e.tensor_tensor(out=dif[k][:], in0=v3(ps[pbk][:]), in1=sm_[:, 0, hs].unsqueeze(2).to_broadcast([128, 8, 64]), op=ALU.subtract), big=True)
                    yield
                    cx.op("act", [difb[k]], [difb[k]], lambda e: e.activation(out=dif[k][:], in_=dif[k][:], func=AF.Exp))
                    yield
                    cx.op("dve", [difb[k], CBmb[i2]], [Mt4b[i2][g]], lambda e: e.scalar_tensor_tensor(out=Mt4[i2][:, hs, :], in0=dif[k][:], scalar=1.0, in1=CBm[i2][:, g, :].unsqueeze(1).to_broadcast([128, 8, 64]),
                                                                                                  op0=ALU.min, op1=ALU.mult))
                    yield

            def grp(t, g, k):
                i2 = t % 2
                i3 = t % 3
                xc_ = xct[i3]
                xcb = xctb[i3]
                bk = 4 + g
                hs = slice(g * 8, (g + 1) * 8)
                gcols = slice(g * 512, (g + 1) * 512)
                sm_ = sm[i2]
                v3 = lambda ap: ap.rearrange("p (h q) -> p h q", q=64)
                with cx.group("pe", [Mt4b[i2][g], xdt4b[i2][g], xtokb[i2], pb_], [psb[bk]]) as box:
                    for hh in range(8):
                        for hf in range(2):
                            rows = slice(hf * 64, (hf + 1) * 64)
                            o_ = ps[bk][rows, hh * 64:(hh + 1) * 64]
                            box.append(nc.tensor.matmul(o_, lhsT=Mt4[i2][rows, g * 8 + hh, :], rhs=xdt4[i2][rows, g * 512 + hh * 64: g * 512 + (hh + 1) * 64], start=True, stop=False))
                            box.append(nc.tensor.matmul(o_, lhsT=Dsk[rows, g * 8 + hh, :], rhs=xtok[i2][rows, g * 512 + hh * 64: g * 512 + (hh + 1) * 64], start=False, stop=True))
                cx.op("act", [psb[bk]], [y1sb[k]], lambda e: e.copy(out=y1s[k][:], in_=ps[bk][:]))
                yield
                for hf in range(2):
                    rows = slice(hf * 64, (hf + 1) * 64)
                    cc = slice(hf * 64, hf * 64 + 64)
                    cx.op("pe", [xcb, stbb[g]], [psb[bk]], lambda e: e.matmul(ps[bk][rows, :], lhsT=xc_[:, 20 + g, cc], rhs=stb[:, g, :], start=True, stop=True))
                    cx.op("dve", [psb[bk], smb[i2]], [tyb[k]], lambda e: e.tensor_tensor(out=v3(ty[k][rows, :]), in0=v3(ps[bk][rows, :]), in1=sm_[rows, 1, hs].unsqueeze(2).to_broadcast([64, 8, 64]), op=ALU.mult), big=True)
                    yield
                    cx.op("pe", [Btokb[i2], xw4b[i2][g]], [psb[bk]], lambda e: e.matmul(ps[bk][:], lhsT=Btok[i2][rows, g * 128:(g + 1) * 128], rhs=xw4[i2][rows, gcols], start=True, stop=True))
                    cx.op("pool", [smb[i2]], [stfb[g]], lambda e: e.tensor_tensor(out=v3(stf[:, g, :]), in0=v3(stf[:, g, :]), in1=sm_[:, 4 + hf, hs].unsqueeze(2).to_broadcast([128, 8, 64]), op=ALU.mult))
                    cx.op("dve", [psb[bk]], [stfb[g]], lambda e: e.tensor_tensor(out=stf[:, g, :], in0=ps[bk][:], in1=stf[:, g, :], op=ALU.add), big=True)
                    cx.op("act", [stfb[g]], [stbb[g]], lambda e: e.copy(out=stb[:, g, :], in_=stf[:, g, :]))
                    yield
                cx.op("pool", [y1sb[k], tyb[k]], [tyb[k]], lambda e: e.tensor_tensor(out=ty[k][:], in0=y1s[k][:], in1=ty[k][:], op=ALU.add))
                cx.op("dve", [tyb[k], szttb[i3]], [yzb[k]], lambda e: e.tensor_tensor(out=yz[k][:], in0=ty[k][:], in1=sztt[i3][:, gcols], op=ALU.mult))
                yield
                cx.op("act", [yzb[k]], [jkb[k], ssqb[k]], lambda e: e.activation(out=jk[k][:], in_=yz[k][:], func=AF.Square, accum_out=ssq[k][:, 0:1]))
                cx.op("act", [ssqb[k]], [ssqb[k]], lambda e: e.activation(out=ssq[k][:, 1:2], in_=ssq[k][:, 0:1], func=AF.Sqrt, bias=self.eps_t[:, 0:1], scale=1.0 / 512.0))
                cx.op("dve", [ssqb[k]], [ssqb[k]], lambda e: e.reciprocal(out=ssq[k][:, 1:2], in_=ssq[k][:, 1:2]))
                cx.op("dve", [yzb[k], ssqb[k]], [ynb[i2]], lambda e: e.tensor_scalar(out=yn[i2][:, gcols], in0=yz[k][:], scalar1=ssq[k][:, 1:2], scalar2=None, op0=ALU.mult))
                yield

            def slot(t, k):
                for g in (k, k + 2):
                    yield from grp(t, g, k)

            def post(t):
                i2 = t % 2
                bi = (t // 4) % 2
                tcols = slice((t % 4) * 128, (t % 4 + 1) * 128)
                for hbk in range(2):
                    pbk = rot[0] % 3
                    rot[0] += 1
                    pv = ps[pbk][:].bitcast(BF16)
                    with cx.group("pe", [ynb[i2], self.cbuf], [psb[pbk]]) as box:
                        for q in range(8):
                            box.append(nc.tensor.transpose(out=pv[:, q * 128:(q + 1) * 128], in_=yn[i2][:, (hbk * 8 + q) * 128:(hbk * 8 + q + 1) * 128], identity=self.ident_b[:]))
                    cx.op("act", [psb[pbk]], [ynTb[bi]], lambda e: e.copy(out=ynT[bi][:, hbk * 8:(hbk + 1) * 8, tcols], in_=pv[:, 0:1024].rearrange("p (q t) -> p q t", t=128)))
                    yield

            def outproj_prefetch(blk):
                bcols = slice(blk * TB, (blk + 1) * TB)
                for fc in range(4):
                    cx.dma("sp", xch[fc][:], self.xs[fc, :, bcols], [self.xs_buf[blk]], [xchb[fc]], xchb[fc])

            def outproj(blk):
                bi = blk % 2
                bcols = slice(blk * TB, (blk + 1) * TB)
                for fc in range(8):
                    pb = rot[0] % 3
                    rot[0] += 1
                    xi = fc % 4
                    if fc >= 4:
                        cx.dma("sp", xch[xi][:], self.xs[fc, :, bcols], [self.xs_buf[blk]], [xchb[xi]], xchb[xi])
                    with cx.group("pe", [wob, ynTb[bi]], [psb[pb]]) as box:
                        for c in range(16):
                            box.append(nc.tensor.matmul(ps[pb][:], lhsT=w_out[:, c, fc * 128:(fc + 1) * 128], rhs=ynT[bi][:, c, :], start=(c == 0), stop=(c == 15)))
                    cx.op("dve", [psb[pb], self.mod_buf[1]], [xchb[xi]], lambda e: e.scalar_tensor_tensor(out=xch[xi][:], in0=ps[pb][:], scalar=self.mod[:, 1, 16 + fc:17 + fc],
                                                                                                         in1=xch[xi][:], op0=ALU.mult, op1=ALU.add))
                    cx.dma("sp", self.xs[fc, :, bcols], xch[xi][:], [xchb[xi]], [self.xs_buf[blk]], self.xs_buf[blk])
                    yield

            load(0)
            if NTL > 1:
                load(1)
            run_threads([pre(0)])
            pending = []
            for t in range(NTL):
                if t + 2 < NTL:
                    load(t + 2)
                th = []
                wts = []
                if t + 1 < NTL:
                    th.append(pre(t + 1))
                    wts.append(12)
                th += [grp(t, g, g) for g in range(4)]
                wts += [1, 1, 1, 1]
                if t > 0:
                    th.append(post(t - 1))
                    wts.append(1)
                th += pending
                wts += [1] * len(pending)
                pending = []
                run_threads(th, wts)
                if t > 0 and (t - 1) % 4 == 3:
                    outproj_prefetch((t - 1) // 4)
                    pending.append(outproj((t - 1) // 4))
            run_threads([post(NTL - 1)] + pending)
            outproj_prefetch((NTL - 1) // 4)
            run_threads([outproj((NTL - 1) // 4)])
            cx.barrier(self.xs_buf + xchb + xctb + szttb + dttb + [wob, pb_, scr])


def _prep_inputs(inputs, b, ntok):
    f = np.float32
    g = lambda k: np.asarray(inputs[k], dtype=f)
    col = lambda v, n: np.ascontiguousarray(v.reshape(n, 128).T)
    m = {}
    m["x"] = np.ascontiguousarray(g("x")[b, :ntok])
    m["c_col"] = col(g("c")[b], 8)
    m["ada_w"] = g("ada_w")
    m["ada_b"] = np.ascontiguousarray(g("ada_b").reshape(2, 1, 6 * D))
    m["norm_mix"] = np.stack([col(g("norm_mix")[i], 8) for i in range(2)])
    m["norm_ffn"] = np.stack([col(g("norm_ffn")[i], 8) for i in range(2)])
    m["norm_final"] = col(g("norm_final"), 8)
    m["gla_w_in"] = g("gla_w_in")[0]
    m["gla_w_gate2"] = g("gla_w_gate2")[0]
    m["gla_b_gate2"] = col(g("gla_b_gate2")[0], 4)
    m["gla_norm"] = col(g("gla_norm")[0], 2)
    m["gla_w_out"] = g("gla_w_out")[0]
    m["ssd_w_in"] = g("ssd_w_in")[0]
    cw = g("ssd_conv_w")[0]
    m["ssd_conv_w"] = np.ascontiguousarray(cw.reshape(4, 24, 128).transpose(2, 1, 0))
    m["ssd_conv_b"] = col(g("ssd_conv_b")[0], 24)
    m["ssd_dt_bias"] = np.ascontiguousarray(g("ssd_dt_bias")[0].reshape(32, 1))
    m["ssd_a_log"] = np.ascontiguousarray(g("ssd_a_log")[0].reshape(32, 1))
    m["ssd_d_bc"] = np.ascontiguousarray(np.broadcast_to(g("ssd_d")[0][None, :], (128, 32)))
    m["ssd_norm_col"] = col(g("ssd_norm")[0], 16)
    m["ssd_w_out"] = g("ssd_w_out")[0]
    m["router_w"] = g("router_w")
    m["router_b_bc"] = np.ascontiguousarray(np.broadcast_to(g("router_b")[None, :], (128, 16)))
    m["moe_w_gate"] = g("moe_w_gate")
    m["moe_w_up"] = g("moe_w_up")
    m["moe_w_down"] = g("moe_w_down")
    return m


def kernel(**inputs):
    ntok = inputs["x"].shape[1]
    nb = inputs["x"].shape[0]
    k = K(ntok)
    nc = k.build()
    in_maps = [_prep_inputs(inputs, b, ntok) for b in range(nb)]
    res = run_bass_kernel_spmd(nc, in_maps, core_ids=list(range(nb)))
    return np.stack([np.asarray(r["out"]) for r in res.results], axis=0).astype(np.float32)
```
